# Optimizing a Trainium2 kernel written in Bass

```python
import jax, jax.numpy as jnp
from jax import lax
import numpy as np

D_MODEL = 1024
BATCH = 16
SEQ = 2048
DEPTH = 4

N_A_LAYERS = DEPTH // 2
N_B_LAYERS = DEPTH - N_A_LAYERS

GLA_HEADS = 4
GLA_DK = D_MODEL // 2 // GLA_HEADS
GLA_DV = D_MODEL // GLA_HEADS
GLA_QK = GLA_HEADS * GLA_DK
GLA_V = GLA_HEADS * GLA_DV
GLA_RANK = 16
GLA_TAU = 16.0
GLA_CHUNK = 64
GLA_IN = 2 * GLA_QK + 2 * GLA_V + GLA_RANK

FOX_HEAD_DIM = 64
FOX_HEADS = D_MODEL // FOX_HEAD_DIM
Q_BLOCK = 128
KV_OUT = 2 * D_MODEL + FOX_HEADS

D_FF = -(-8 * D_MODEL // (3 * 256)) * 256
EPS = 1e-6

kernel_name = "yoco_gla_fox_hybrid"


def rmsnorm(x, g):
    xf = x.astype(jnp.float32)
    y = xf * lax.rsqrt(jnp.mean(xf * xf, axis=-1, keepdims=True) + EPS) * g.astype(jnp.float32)
    return y.astype(x.dtype)


def swiglu(h, w_gu, w_down):
    gate, up = jnp.split(h @ w_gu, 2, axis=-1)
    return (jax.nn.silu(gate) * up) @ w_down


def gla_mixer(h, w_in, w_alpha_up, b_alpha, g_head, w_out):
    B, T, _ = h.shape
    NC, C = T // GLA_CHUNK, GLA_CHUNK
    proj = h @ w_in
    q, k, v, r, a_low = jnp.split(
        proj, [GLA_QK, 2 * GLA_QK, 2 * GLA_QK + GLA_V, 2 * GLA_QK + 2 * GLA_V], axis=-1)
    log_alpha = jax.nn.log_sigmoid((a_low @ w_alpha_up + b_alpha).astype(jnp.float32)) / GLA_TAU

    def heads(t, d):
        return t.astype(jnp.float32).reshape(B, NC, C, GLA_HEADS, d).transpose(0, 3, 1, 2, 4)

    q = heads(q, GLA_DK) * (GLA_DK ** -0.5)
    k = heads(k, GLA_DK)
    v = heads(v, GLA_DV)
    b = jnp.cumsum(heads(log_alpha, GLA_DK), axis=3)
    b_last = b[:, :, :, -1:, :]
    q_dec = q * jnp.exp(b)
    k_dec = k * jnp.exp(-b)
    causal = jnp.tril(jnp.ones((C, C), dtype=bool))
    A = jnp.where(causal, jnp.einsum('bhncd,bhnsd->bhncs', q_dec, k_dec), 0.0)
    o_intra = jnp.einsum('bhncs,bhnsv->bhncv', A, v)
    chunk_kv = jnp.einsum('bhncd,bhncv->bhndv', k * jnp.exp(b_last - b), v)
    chunk_decay = jnp.exp(b_last[:, :, :, 0, :])

    def step(S, inp):
        decay, kv = inp
        return decay[..., None] * S + kv, S

    S0 = jnp.zeros((B, GLA_HEADS, GLA_DK, GLA_DV), jnp.float32)
    _, S_prev = lax.scan(step, S0, (jnp.moveaxis(chunk_decay, 2, 0), jnp.moveaxis(chunk_kv, 2, 0)))
    S_prev = jnp.moveaxis(S_prev, 0, 2)
    o = o_intra + jnp.einsum('bhncd,bhndv->bhncv', q_dec, S_prev)
    o = o.transpose(0, 2, 3, 1, 4).reshape(B, T, GLA_HEADS, GLA_DV)
    o = rmsnorm(o, g_head).reshape(B, T, GLA_V)
    o = o * jax.nn.silu(r.astype(jnp.float32))
    return o.astype(h.dtype) @ w_out


def shared_kv(h, kv_norm, w_kv, b_f):
    B, T, _ = h.shape
    proj = rmsnorm(h, kv_norm) @ w_kv
    k, v, f_logit = jnp.split(proj, [D_MODEL, 2 * D_MODEL], axis=-1)
    k = k.reshape(B, T, FOX_HEADS, FOX_HEAD_DIM).transpose(0, 2, 1, 3)
    v = v.reshape(B, T, FOX_HEADS, FOX_HEAD_DIM).transpose(0, 2, 1, 3)
    log_f = jax.nn.log_sigmoid(f_logit.astype(jnp.float32) + b_f.astype(jnp.float32))
    c = jnp.cumsum(log_f, axis=1).transpose(0, 2, 1)
    return k, v, c


def fox_mixer(h, k, v, c, w_q, w_o):
    B, T, _ = h.shape
    q = (h @ w_q).reshape(B, T, FOX_HEADS, FOX_HEAD_DIM).transpose(0, 2, 1, 3)
    q = q * (FOX_HEAD_DIM ** -0.5)
    outs = []
    for blk in range(T // Q_BLOCK):
        s0, e = blk * Q_BLOCK, (blk + 1) * Q_BLOCK
        logits = jnp.einsum('bhqd,bhkd->bhqk', q[:, :, s0:e], k[:, :, :e]).astype(jnp.float32)
        logits = logits + c[:, :, s0:e, None] - c[:, :, None, :e]
        mask = (s0 + jnp.arange(Q_BLOCK))[:, None] >= jnp.arange(e)[None, :]
        p = jax.nn.softmax(jnp.where(mask, logits, -jnp.inf), axis=-1).astype(v.dtype)
        outs.append(jnp.einsum('bhqk,bhkd->bhqd', p, v[:, :, :e]))
    o = jnp.concatenate(outs, axis=2).transpose(0, 2, 1, 3).reshape(B, T, D_MODEL)
    return o @ w_o


def setup_inputs(seed: int = 0) -> dict:
    key = jax.random.key(seed)
    ks = jax.random.split(key, 20)
    f32 = jnp.float32
    D = D_MODEL
    res_scale = (2.0 * DEPTH) ** -0.5

    def nrm(k, shape, scale):
        return jax.random.normal(k, shape, f32) * scale

    def gain(k, shape):
        return 1.0 + 0.01 * jax.random.normal(k, shape, f32)

    x = jax.random.normal(ks[0], (BATCH, SEQ, D), f32)
    attn_norm = gain(ks[1], (DEPTH, D))
    ffn_norm = gain(ks[2], (DEPTH, D))
    gla_w_in = nrm(ks[3], (N_A_LAYERS, D, GLA_IN), D ** -0.5)
    gla_w_alpha_up = nrm(ks[4], (N_A_LAYERS, GLA_RANK, GLA_QK), GLA_RANK ** -0.5)
    gla_b_alpha = nrm(ks[5], (N_A_LAYERS, GLA_QK), 0.1)
    gla_g_head = gain(ks[6], (N_A_LAYERS, GLA_DV))
    gla_w_out = nrm(ks[7], (N_A_LAYERS, GLA_V, D), GLA_V ** -0.5 * res_scale)
    kv_norm = gain(ks[8], (D,))
    w_kv = jnp.concatenate([
        nrm(ks[9], (D, 2 * D), D ** -0.5),
        nrm(ks[10], (D, FOX_HEADS), 0.1 * D ** -0.5)], axis=-1)
    b_f = 3.0 + nrm(ks[11], (FOX_HEADS,), 0.1)
    fox_w_q = nrm(ks[12], (N_B_LAYERS, D, D), D ** -0.5)
    fox_w_o = nrm(ks[13], (N_B_LAYERS, D, D), D ** -0.5 * res_scale)
    ffn_w_gu = nrm(ks[14], (DEPTH, D, 2 * D_FF), D ** -0.5)
    ffn_w_down = nrm(ks[15], (DEPTH, D_FF, D), D_FF ** -0.5 * res_scale)
    final_norm = gain(ks[16], (D,))
    return {"x": x, "attn_norm": attn_norm, "ffn_norm": ffn_norm,
            "gla_w_in": gla_w_in, "gla_w_alpha_up": gla_w_alpha_up, "gla_b_alpha": gla_b_alpha,
            "gla_g_head": gla_g_head, "gla_w_out": gla_w_out,
            "kv_norm": kv_norm, "w_kv": w_kv, "b_f": b_f,
            "fox_w_q": fox_w_q, "fox_w_o": fox_w_o,
            "ffn_w_gu": ffn_w_gu, "ffn_w_down": ffn_w_down, "final_norm": final_norm}


def reference(x, attn_norm, ffn_norm, gla_w_in, gla_w_alpha_up, gla_b_alpha, gla_g_head,
              gla_w_out, kv_norm, w_kv, b_f, fox_w_q, fox_w_o, ffn_w_gu, ffn_w_down, final_norm):
    h = x
    k_sh = v_sh = c_sh = None
    for layer in range(DEPTH):
        if layer < N_A_LAYERS:
            h = h + gla_mixer(rmsnorm(h, attn_norm[layer]), gla_w_in[layer], gla_w_alpha_up[layer],
                              gla_b_alpha[layer], gla_g_head[layer], gla_w_out[layer])
        else:
            if layer == N_A_LAYERS:
                k_sh, v_sh, c_sh = shared_kv(h, kv_norm, w_kv, b_f)
            j = layer - N_A_LAYERS
            h = h + fox_mixer(rmsnorm(h, attn_norm[layer]), k_sh, v_sh, c_sh, fox_w_q[j], fox_w_o[j])
        h = h + swiglu(rmsnorm(h, ffn_norm[layer]), ffn_w_gu[layer], ffn_w_down[layer])
    return rmsnorm(h, final_norm)
```

```python
import numpy as np
from contextlib import ExitStack
import concourse.bass as bass
import concourse.mybir as mybir
from concourse.bass_utils import run_bass_kernel_spmd

F32 = mybir.dt.float32
BF16 = mybir.dt.bfloat16
AF = mybir.ActivationFunctionType
ALU = mybir.AluOpType
AX = mybir.AxisListType

ENGS = ["sync", "scalar", "vector", "gpsimd", "tensor"]


class Buf:
    __slots__ = ("name", "w", "rs", "dsem")

    def __init__(self, name):
        self.name = name
        self.w = None
        self.rs = {}
        self.dsem = None


class Op:
    __slots__ = ("eng", "fn", "deps", "observed", "count", "is_dma", "dsem")


class Sched:
    def __init__(self):
        self.ops = {e: [] for e in ENGS}
        self.n_dsem = 0
        self.last_on_dsem = {}
        self.dma_ops = []
        self.dsem_by_name = {}
        self.nops = 0

    def add(self, eng, fn, reads=(), writes=(), dma=False, dsem=None):
        op = Op()
        op.eng = eng
        op.fn = fn
        op.is_dma = dma
        op.dsem = dsem
        op.observed = False
        op.count = None
        deps = {}
        for b in reads:
            if b.w is not None:
                deps[id(b.w)] = b.w
        for b in writes:
            if b.w is not None:
                deps[id(b.w)] = b.w
            for r in b.rs.values():
                deps[id(r)] = r
        if dma:
            prev = self.last_on_dsem.get(dsem)
            if prev is not None:
                deps[id(prev)] = prev
            self.last_on_dsem[dsem] = op
            self.dma_ops.append(op)
        dl = []
        for d in deps.values():
            if d is op:
                continue
            if eng == "tensor" and d.eng == "tensor" and not d.is_dma and not dma:
                continue
            d.observed = True
            dl.append(d)
        op.deps = dl
        key = ("d", dsem) if dma else ("e", eng)
        for b in reads:
            b.rs[key] = op
        for b in writes:
            b.w = op
            b.rs = {}
        self.ops[eng].append(op)
        self.nops += 1
        return op

    def new_dsem(self):
        self.n_dsem += 1
        return self.n_dsem - 1

    def dma(self, eng, out_ap, in_ap, sb, reads=(), writes=()):
        if sb.dsem is None:
            if sb.name not in self.dsem_by_name:
                self.dsem_by_name[sb.name] = self.new_dsem()
            sb.dsem = self.dsem_by_name[sb.name]
        return self.add(eng, lambda e, o=out_ap, i=in_ap: e.dma_start(out=o, in_=i),
                        reads=reads, writes=writes, dma=True, dsem=sb.dsem)

    def emit(self, nc, stack):
        esem = {e: stack.enter_context(nc.semaphore("pg_" + e)) for e in ENGS}
        dsem = [stack.enter_context(nc.semaphore("dm_%d" % i)) for i in range(self.n_dsem)]
        for e in ENGS:
            c = 0
            for op in self.ops[e]:
                if not op.is_dma and op.observed:
                    c += 1
                    op.count = c
        dc = [0] * self.n_dsem
        for op in self.dma_ops:
            dc[op.dsem] += 16
            op.count = dc[op.dsem]
        block = stack.enter_context(nc.Block())

        def run(e_name):
            def body(eng):
                waited = {}
                for op in self.ops[e_name]:
                    need = {}
                    for d in op.deps:
                        k = ("d", d.dsem) if d.is_dma else ("e", d.eng)
                        if need.get(k, 0) < d.count:
                            need[k] = d.count
                    for k, v in need.items():
                        if waited.get(k, 0) >= v:
                            continue
                        waited[k] = v
                        s = dsem[k[1]] if k[0] == "d" else esem[k[1]]
                        eng.wait_ge(s, v)
                    if op.fn is None:
                        continue
                    ins = op.fn(eng)
                    if op.is_dma:
                        ins.then_inc(dsem[op.dsem], 16)
                    elif op.observed:
                        ins.then_inc(esem[e_name], 1)
                if e_name == "sync":
                    for i in range(self.n_dsem):
                        if dc[i] > 0:
                            eng.wait_ge(dsem[i], dc[i])
            return body

        block.sync(run("sync"))
        block.scalar(run("scalar"))
        block.vector(run("vector"))
        block.gpsimd(run("gpsimd"))
        block.tensor(run("tensor"))

    def barrier(self):
        lasts = []
        for e in ENGS:
            for op in reversed(self.ops[e]):
                if not op.is_dma and op.fn is not None:
                    lasts.append(op)
                    break
        lasts.extend(self.last_on_dsem.values())
        for e in ENGS:
            op = Op()
            op.eng = e
            op.fn = None
            op.is_dma = False
            op.dsem = None
            op.observed = False
            op.count = None
            op.deps = list(lasts)
            for d in op.deps:
                d.observed = True
            self.ops[e].append(op)


D = 1024
NCK = 8
DFF = 2816
NJ = 22
NSLAB = 11
GROUPS = [(0, 3), (3, 6), (6, 9), (9, 11)]
EPS = 1e-6
GLA_SCALE = 128 ** -0.5

GC_ATTN = 0
GC_FFN = 32
GC_KV = 64
GC_FINAL = 72
GC_BALPHA = 80
GC_GHEAD = 88
NGC = 92


def build_program(T=2048, NSEQ=2, n_gla=2, n_fox=2, do_final=True):
    NB = T // 512
    NT = T // 128
    nc = bass.Bass("TRN2", target_bir_lowering=False)
    S = Sched()
    dt = lambda name, shape, dty, kind: nc.dram_tensor(name, shape, dty, kind=kind).ap()
    xT_d = dt("xT", [NSEQ, 128, NCK, T], F32, "ExternalInput")
    gcol_d = dt("gcol", [128, NGC], F32, "ExternalInput")
    bfb_d = dt("bfb", [128, 16], F32, "ExternalInput")
    wgla_d = dt("wgla", [2, 4, 128, NCK, 768], F32, "ExternalInput")
    wout_d = dt("wout", [2, 4, 128, 2, 1024], F32, "ExternalInput")
    wa_d = dt("wa", [2, 128, NCK, 16], F32, "ExternalInput")
    wup_d = dt("wup", [2, 16, 512], F32, "ExternalInput")
    wgu_d = dt("wgu", [4, NSLAB, 128, NCK, 512], F32, "ExternalInput")
    wd_d = dt("wd", [4, 128, NJ, 1024], F32, "ExternalInput")
    wk_d = dt("wk", [8, 128, NCK, 128], F32, "ExternalInput")
    wv_d = dt("wv", [128, NCK, 1024], F32, "ExternalInput")
    wf_d = dt("wf", [128, NCK, 16], F32, "ExternalInput")
    wq_d = dt("wq", [2, 8, 128, NCK, 128], F32, "ExternalInput")
    wo_d = dt("wo", [2, 128, NCK, 1024], F32, "ExternalInput")
    yT_d = dt("yT", [NSEQ, 128, NCK, T], F32, "ExternalOutput")
    kT_d = dt("kT_s", [8, 128, T], BF16, "Internal")
    v_d = dt("v_s", [T, 2048], BF16, "Internal")
    Bk_d = [Buf("kTd%d" % i) for i in range(8)]
    Bv_d = Buf("vd")

    st = ExitStack()
    with st:
        sb = lambda name, shape, dty: st.enter_context(nc.sbuf_tensor(name, shape, dty))
        hT = sb("hT", [128, NCK, T], F32)
        hnT = sb("hnT", [128, NCK, T], BF16)
        ones_bf = sb("ones_bf", [128, 128], BF16)
        ident_bf = sb("ident_bf", [128, 128], BF16)
        maskbd = sb("maskbd", [128, 512], BF16)
        tri = sb("tri", [128, 128], BF16)
        scanmask = sb("scanmask", [128, 512], F32)
        U32 = sb("U32", [128, 128], F32)
        ones32 = sb("ones32", [128, 128], F32)
        E032 = sb("E032", [128, 128], F32)
        gcol = sb("gcol_s", [128, NGC], F32)
        nbal = sb("nbal", [128, 8], F32)
        bfb = sb("bfb_s", [128, 16], F32)
        sq = [sb("sq%d" % i, [128, 512], BF16) for i in range(4)]
        rs_t = [sb("rs%d" % i, [128, 512], F32) for i in range(2)]
        rstd_t = [sb("rstd%d" % i, [128, 512], F32) for i in range(2)]
        ctok = sb("ctok", [128, NT, 16], F32)
        cref = sb("cref", [128, NB, 16], F32)
        bias_t = sb("bias_t", [128, NT, NB, 16], F32)
        RB = 86 * 1024
        R = sb("R", [128, RB], mybir.dt.uint8)
        ps = [st.enter_context(nc.psum_tensor("ps%d" % i, [128, 512], F32)) for i in range(8)]
        PB = [Buf("ps%d" % i) for i in range(8)]

        class Carver:
            def __init__(self):
                self.off = 0

            def take(self, shape, dty):
                esz = 2 if dty == BF16 else 4
                n = int(np.prod(shape[1:])) * esz
                o = (self.off + 31) // 32 * 32
                assert o + n <= RB, ("region overflow", o + n, RB)
                self.off = o + n
                v = R[0:shape[0], o:o + n].bitcast(dty)
                if len(shape) == 3:
                    v = v.rearrange("p (a b) -> p a b", b=shape[2])
                elif len(shape) == 4:
                    v = v.rearrange("p (a b c) -> p a b c", b=shape[2], c=shape[3])
                return v

        bank_rr = [0]

        def bank(lo=0, hi=8):
            i = bank_rr[0] % (hi - lo) + lo
            bank_rr[0] += 1
            return i

        def mm(out, lhsT, rhs, start, stop, reads, writes):
            S.add("tensor", lambda e: e.matmul(out, lhsT, rhs, start=start, stop=stop),
                  reads=reads, writes=writes)

        Bh = [[Buf("h%d_%d" % (c, b)) for b in range(NB)] for c in range(NCK)]
        Bhn = [[Buf("hn%d_%d" % (c, b)) for b in range(NB)] for c in range(NCK)]
        Bconst = Buf("const")
        Bsq = [Buf("sq%d" % i) for i in range(4)]
        Brs = [Buf("rs%d" % i) for i in range(2)]
        Brstd = [Buf("rstd%d" % i) for i in range(2)]
        Bctok = Buf("ctok")
        Bcref = Buf("cref")
        Bbias = Buf("bias")
        tbs = lambda b: slice(b * 512, (b + 1) * 512)

        def consts():
            G = "gpsimd"
            S.dma("sync", gcol[:], gcol_d, Bconst, writes=[Bconst])
            S.dma("sync", bfb[:], bfb_d, Bconst, writes=[Bconst])
            S.add(G, lambda e: e.memset(ones_bf[:], 1.0), writes=[Bconst])
            S.add(G, lambda e: e.memset(ones32[:], 1.0), writes=[Bconst])
            S.add(G, lambda e: e.affine_select(out=ident_bf[:], in_=ones_bf[:], pattern=[[-1, 128]],
                                               compare_op=ALU.is_equal, fill=0.0, base=0,
                                               channel_multiplier=1), reads=[Bconst], writes=[Bconst])
            S.add(G, lambda e: e.affine_select(out=tri[:], in_=ones_bf[:], pattern=[[1, 128]],
                                               compare_op=ALU.is_ge, fill=0.0, base=0,
                                               channel_multiplier=-1), reads=[Bconst], writes=[Bconst])
            S.add(G, lambda e: e.affine_select(out=U32[:], in_=ones32[:], pattern=[[1, 128]],
                                               compare_op=ALU.is_ge, fill=0.0, base=0,
                                               channel_multiplier=-1), reads=[Bconst], writes=[Bconst])
            S.add(G, lambda e: e.affine_select(out=E032[:], in_=ones32[:], pattern=[[0, 128]],
                                               compare_op=ALU.is_ge, fill=0.0, base=0,
                                               channel_multiplier=-1), reads=[Bconst], writes=[Bconst])
            for r in range(4):
                S.add(G, lambda e, r=r: e.tensor_copy(out=maskbd[:, r * 128:(r + 1) * 128], in_=tri[:]),
                      reads=[Bconst], writes=[Bconst])
                S.add(G, lambda e, r=r: e.memset(maskbd[0:64, r * 128 + 64:(r + 1) * 128], 0.0),
                      writes=[Bconst])
            S.add(G, lambda e: e.memset(scanmask[:], 1.0), writes=[Bconst])
            S.add(G, lambda e: e.memset(scanmask[:].rearrange("p (c k) -> p c k", k=64)[:, :, 0:1], 0.0),
                  writes=[Bconst])
            S.add(G, lambda e: e.tensor_scalar(out=nbal[:], in0=gcol[:, GC_BALPHA:GC_BALPHA + 8], scalar1=-1.0,
                                               scalar2=None, op0=ALU.mult), reads=[Bconst], writes=[Bconst])

        nrm_ctr = [0]

        def norm(gbase, final_seq=None, ystage=None, Bystage=None):
            for b in range(NB):
                k = nrm_ctr[0] % 2
                nrm_ctr[0] += 1
                bk = bank()
                for c in range(NCK):
                    s4 = (b * NCK + c) % 4
                    S.add("scalar", lambda e, c=c, s4=s4, b=b: e.activation(out=sq[s4][:], in_=hT[:, c, tbs(b)], func=AF.Square),
                          reads=[Bh[c][b]], writes=[Bsq[s4]])
                    mm(ps[bk][:], ones_bf[:], sq[s4][:], c == 0, c == NCK - 1, [Bconst, Bsq[s4]], [PB[bk]])
                S.add("scalar", lambda e, k=k, bk=bk: e.activation(out=rs_t[k][:], in_=ps[bk][:], func=AF.Ln, scale=1.0 / D, bias=EPS),
                      reads=[PB[bk]], writes=[Brs[k]])
                S.add("scalar", lambda e, k=k: e.activation(out=rstd_t[k][:], in_=rs_t[k][:], func=AF.Exp, scale=-0.5), reads=[Brs[k]], writes=[Brstd[k]])
                for c in range(NCK):
                    if final_seq is None:
                        eng = "vector"
                        S.add(eng, lambda e, c=c, k=k, b=b: e.scalar_tensor_tensor(
                            out=hnT[:, c, tbs(b)], in0=hT[:, c, tbs(b)], scalar=gcol[:, gbase + c:gbase + c + 1],
                            in1=rstd_t[k][:], op0=ALU.mult, op1=ALU.mult),
                            reads=[Bh[c][b], Brstd[k], Bconst], writes=[Bhn[c][b]])
                    else:
                        ys = (b * NCK + c) % len(ystage)
                        S.add("vector", lambda e, c=c, k=k, b=b, ys=ys: e.scalar_tensor_tensor(
                            out=ystage[ys][:], in0=hT[:, c, tbs(b)], scalar=gcol[:, gbase + c:gbase + c + 1],
                            in1=rstd_t[k][:], op0=ALU.mult, op1=ALU.mult),
                            reads=[Bh[c][b], Brstd[k], Bconst], writes=[Bystage[ys]])
                        S.dma("sync", yT_d[final_seq, :, c, tbs(b)], ystage[ys][:], Bystage[ys], reads=[Bystage[ys]])

        def resid_add(c, b, bk):
            S.add("vector", lambda e: e.tensor_tensor(out=hT[:, c, tbs(b)], in0=hT[:, c, tbs(b)], in1=ps[bk][:], op=ALU.add),
                  reads=[Bh[c][b], PB[bk]], writes=[Bh[c][b]])

        def ffn(l):
            cv = Carver()
            act = cv.take([128, 6, T], BF16)
            wgu = [cv.take([128, NCK, 512], BF16) for _ in range(3)]
            wdn = [cv.take([128, 6, 1024], BF16) for _ in range(2)]
            sg = [cv.take([128, 512], BF16) for _ in range(2)]
            ub = [cv.take([128, 512], BF16) for _ in range(2)]
            Bact = [[Buf("act") for _ in range(NB)] for _ in range(6)]
            Bwgu = [Buf("wgu%d" % i) for i in range(3)]
            Bwdn = [Buf("wdn%d" % i) for i in range(2)]
            Bsg = [Buf("sg"), Buf("sg")]
            Bub = [Buf("ub"), Buf("ub")]
            norm(GC_FFN + l * 8)
            ectr = 0
            for gi, (s0, s1) in enumerate(GROUPS):
                nj = (s1 - s0) * 2
                S.dma("gpsimd", wdn[gi % 2][:, 0:nj, :], wd_d[l, :, 2 * s0:2 * s1, :], Bwdn[gi % 2], writes=[Bwdn[gi % 2]])
                for sl in range(s0, s1):
                    w = sl % 3
                    S.dma("gpsimd", wgu[w][:], wgu_d[l, sl], Bwgu[w], writes=[Bwgu[w]])
                    for jj in range(2):
                        jl = (sl - s0) * 2 + jj
                        for b in range(NB):
                            bg, bu = bank(), bank()
                            for c in range(NCK):
                                mm(ps[bg][:], wgu[w][:, c, jj * 128:(jj + 1) * 128], hnT[:, c, tbs(b)], c == 0, c == NCK - 1,
                                   [Bwgu[w], Bhn[c][b]], [PB[bg]])
                            for c in range(NCK):
                                mm(ps[bu][:], wgu[w][:, c, 256 + jj * 128:256 + (jj + 1) * 128], hnT[:, c, tbs(b)], c == 0, c == NCK - 1,
                                   [Bwgu[w], Bhn[c][b]], [PB[bu]])
                            k = ectr % 2
                            ectr += 1
                            S.add("scalar", lambda e, k=k, bg=bg: e.activation(out=sg[k][:], in_=ps[bg][:], func=AF.Silu),
                                  reads=[PB[bg]], writes=[Bsg[k]])
                            S.add("vector", lambda e, k=k, bu=bu, jl=jl, b=b: e.tensor_tensor(out=act[:, jl, tbs(b)], in0=ps[bu][:], in1=sg[k][:], op=ALU.mult),
                                  reads=[PB[bu], Bsg[k]], writes=[Bact[jl][b]])
                for c in range(NCK):
                    for b in range(NB):
                        bk = bank()
                        for jl in range(nj):
                            mm(ps[bk][:], wdn[gi % 2][:, jl, c * 128:(c + 1) * 128], act[:, jl, tbs(b)], jl == 0, jl == nj - 1,
                               [Bwdn[gi % 2], Bact[jl][b]], [PB[bk]])
                        resid_add(c, b, bk)

        def gla(l):
            cv = Carver()
            wsl = [cv.take([128, NCK, 768], BF16) for _ in range(2)]
            wot = [cv.take([128, 2, 1024], BF16) for _ in range(2)]
            wa = cv.take([128, NCK, 16], BF16)
            wup = cv.take([16, 512], BF16)
            alow = cv.take([16, T], BF16)
            Sf = [cv.take([128, 256], F32) for _ in range(2)]
            dec = [cv.take([128, 8], F32) for _ in range(2)]
            P2 = lambda shape, dty: [cv.take(shape, dty) for _ in range(2)]
            lp1 = cv.take([128, 512], F32)
            lp = [lp1, lp1]
            cs1 = cv.take([128, 512], F32)
            cs = [cs1, cs1]
            eb = P2([128, 512], BF16)
            enb = P2([128, 512], BF16)
            qd = P2([128, 512], BF16)
            kd = P2([128, 512], BF16)
            kk = P2([128, 512], BF16)
            kktok = P2([128, 4, 128], BF16)
            vt = P2([128, 4, 256], BF16)
            AT = P2([128, 512], BF16)
            Sall = P2([128, 8, 256], BF16)
            Sfin = [cv.take([128, 256], BF16) for _ in range(3)]
            sr = P2([128, 2, 512], BF16)
            og = P2([128, 2, 512], BF16)
            t0 = og
            names = "wsl wot lp cs eb enb qd kd kk kktok vt AT sr og dec".split()
            BB = {n: [Buf(n + "0"), Buf(n + "1")] for n in names}
            BB["lp"][1] = BB["lp"][0]
            BB["cs"][1] = BB["cs"][0]
            BB["t0"] = BB["og"]
            Bwa, Bwup, Balow = Buf("wa"), Buf("wup"), Buf("alow")
            BSf = [Buf("Sf0"), Buf("Sf1")]
            BSall = [[Buf("Sall") for _ in range(8)] for _ in range(2)]
            BSfin = [Buf("Sfin") for _ in range(3)]
            norm(GC_ATTN + l * 8)
            S.dma("gpsimd", wa[:], wa_d[l], Bwa, writes=[Bwa])
            S.dma("gpsimd", wup[:], wup_d[l], Bwup, writes=[Bwup])
            for b in range(NB):
                bk = bank()
                for c in range(NCK):
                    mm(ps[bk][0:16, :], wa[:, c, :], hnT[:, c, tbs(b)], c == 0, c == NCK - 1, [Bwa, Bhn[c][b]], [PB[bk]])
                S.add("scalar", lambda e, bk=bk, b=b: e.copy(out=alow[:, tbs(b)], in_=ps[bk][0:16, :]), reads=[PB[bk]], writes=[Balow])
            sfi = [0]

            def front(it, h, b):
                k = it % 2
                hs = h % 2
                if b == 0:
                    S.dma("gpsimd", wsl[hs][:], wgla_d[l, h], BB["wsl"][hs], writes=[BB["wsl"][hs]])
                    S.dma("gpsimd", wot[hs][:], wout_d[l, h], BB["wot"][hs], writes=[BB["wot"][hs]])
                W = wsl[hs]
                BW = BB["wsl"][hs]
                rd_hn = [Bhn[c][b] for c in range(NCK)]
                bx = bank()
                mm(ps[bx][:], wup[:, h * 128:(h + 1) * 128], alow[:, tbs(b)], True, True, [Bwup, Balow], [PB[bx]])
                S.add("scalar", lambda e, k=k, bx=bx, h=h: e.activation(out=lp[k][:], in_=ps[bx][:], func=AF.Exp, scale=-1.0,
                                                                     bias=nbal[:, l * 4 + h:l * 4 + h + 1]),
                      reads=[PB[bx], Bconst], writes=[BB["lp"][k]])
                S.add("scalar", lambda e, k=k: e.activation(out=lp[k][:], in_=lp[k][:], func=AF.Ln, bias=1.0),
                      reads=[BB["lp"][k]], writes=[BB["lp"][k]])
                S.add("vector", lambda e, k=k: e.tensor_tensor_scan(out=cs[k][:], data0=scanmask[:], data1=lp[k][:], initial=0.0,
                                                                   op0=ALU.mult, op1=ALU.add),
                      reads=[BB["lp"][k], Bconst], writes=[BB["cs"][k]])
                S.add("scalar", lambda e, k=k: e.activation(out=eb[k][:], in_=cs[k][:], func=AF.Exp, scale=-1.0 / 16),
                      reads=[BB["cs"][k]], writes=[BB["eb"][k]])
                S.add("scalar", lambda e, k=k: e.activation(out=enb[k][:], in_=cs[k][:], func=AF.Exp, scale=1.0 / 16),
                      reads=[BB["cs"][k]], writes=[BB["enb"][k]])
                S.add("scalar", lambda e, k=k: e.activation(out=dec[k][:], in_=cs[k][:].rearrange("p (c k) -> p c k", k=64)[:, :, 63],
                                                            func=AF.Exp, scale=-1.0 / 16),
                      reads=[BB["cs"][k]], writes=[BB["dec"][k]])
                bq, bkk = bank(), bank()
                for c in range(NCK):
                    mm(ps[bq][:], W[:, c, 0:128], hnT[:, c, tbs(b)], c == 0, c == NCK - 1, [BW, rd_hn[c]], [PB[bq]])
                for c in range(NCK):
                    mm(ps[bkk][:], W[:, c, 128:256], hnT[:, c, tbs(b)], c == 0, c == NCK - 1, [BW, rd_hn[c]], [PB[bkk]])
                S.add("vector", lambda e, k=k, bq=bq: e.scalar_tensor_tensor(out=qd[k][:], in0=ps[bq][:], scalar=GLA_SCALE, in1=eb[k][:],
                                                                             op0=ALU.mult, op1=ALU.mult),
                      reads=[PB[bq], BB["eb"][k]], writes=[BB["qd"][k]])
                S.add("vector", lambda e, k=k, bkk=bkk: e.tensor_tensor(out=kd[k][:], in0=ps[bkk][:], in1=enb[k][:], op=ALU.mult),
                      reads=[PB[bkk], BB["enb"][k]], writes=[BB["kd"][k]])
                S.add("gpsimd", lambda e, k=k: e.tensor_tensor(
                    out=kk[k][:].rearrange("p (c k) -> p c k", k=64), in0=kd[k][:].rearrange("p (c k) -> p c k", k=64),
                    in1=dec[k][:, 0:8].unsqueeze(2).to_broadcast([128, 8, 64]), op=ALU.mult),
                    reads=[BB["kd"][k], BB["dec"][k]], writes=[BB["kk"][k]])
                for tt in range(4):
                    bv = bank()
                    tok = slice(b * 512 + tt * 128, b * 512 + (tt + 1) * 128)
                    for c in range(NCK):
                        mm(ps[bv][:, 0:256], hnT[:, c, tok], W[:, c, 256:512], c == 0, c == NCK - 1, [BW, rd_hn[c]], [PB[bv]])
                    S.add("scalar", lambda e, k=k, bv=bv, tt=tt: e.copy(out=vt[k][:, tt, :], in_=ps[bv][:, 0:256]),
                          reads=[PB[bv]], writes=[BB["vt"][k]])
                for vc in range(2):
                    br = bank()
                    for c in range(NCK):
                        mm(ps[br][:], W[:, c, 512 + vc * 128:512 + (vc + 1) * 128], hnT[:, c, tbs(b)], c == 0, c == NCK - 1,
                           [BW, rd_hn[c]], [PB[br]])
                    S.add("scalar", lambda e, k=k, br=br, vc=vc: e.activation(out=sr[k][:, vc, :], in_=ps[br][:], func=AF.Silu),
                          reads=[PB[br]], writes=[BB["sr"][k]])

            def mid(it, h, b):
                k = it % 2
                hs = h % 2
                if b == 0:
                    S.add("gpsimd", lambda e: e.memset(Sf[0][:], 0.0), writes=[BSf[0]])
                    sfi[0] = 0
                bt = bank()
                for tt in range(4):
                    mm(ps[bt][:, tt * 128:(tt + 1) * 128], kk[k][:, tt * 128:(tt + 1) * 128], ident_bf[:], True, True,
                       [BB["kk"][k], Bconst], [PB[bt]])
                S.add("scalar", lambda e, k=k, bt=bt: e.copy(out=kktok[k][:].rearrange("p a b -> p (a b)"), in_=ps[bt][:]),
                      reads=[PB[bt]], writes=[BB["kktok"][k]])
                ba = bank()
                for tt in range(4):
                    mm(ps[ba][:, tt * 128:(tt + 1) * 128], kd[k][:, tt * 128:(tt + 1) * 128], qd[k][:, tt * 128:(tt + 1) * 128], True, True,
                       [BB["kd"][k], BB["qd"][k]], [PB[ba]])
                S.add("vector", lambda e, k=k, ba=ba: e.tensor_tensor(out=AT[k][:], in0=ps[ba][:], in1=maskbd[:], op=ALU.mult),
                      reads=[PB[ba], Bconst], writes=[BB["AT"][k]])
                for ci in range(8):
                    tt, hf = ci // 2, ci % 2
                    rows = slice(64 * hf, 64 * hf + 64)
                    bkv = bank()
                    mm(ps[bkv][:, 0:256], kktok[k][rows, tt, :], vt[k][rows, tt, :], True, True,
                       [BB["kktok"][k], BB["vt"][k]], [PB[bkv]])
                    so, sn = sfi[0], 1 - sfi[0]
                    S.add("vector", lambda e, k=k, ci=ci, bkv=bkv, so=so, sn=sn: e.scalar_tensor_tensor(
                        out=Sf[sn][:], in0=Sf[so][:], scalar=dec[k][:, ci:ci + 1], in1=ps[bkv][:, 0:256], op0=ALU.mult, op1=ALU.add),
                        reads=[BSf[so], BB["dec"][k], PB[bkv]], writes=[BSf[sn]])
                    sfi[0] = sn
                    if ci < 7:
                        S.add("scalar", lambda e, k=k, ci=ci, sn=sn: e.copy(out=Sall[k][:, ci + 1, :], in_=Sf[sn][:]),
                              reads=[BSf[sn]], writes=[BSall[k][ci + 1]])
                    elif b < NB - 1:
                        S.add("scalar", lambda e, it=it, sn=sn: e.copy(out=Sfin[it % 3][:], in_=Sf[sn][:]),
                              reads=[BSf[sn]], writes=[BSfin[it % 3]])

            def out(it, h, b):
                k = it % 2
                hs = h % 2
                bo = [bank(), bank()]
                for vc in range(2):
                    for tt in range(4):
                        mm(ps[bo[vc]][:, tt * 128:(tt + 1) * 128], vt[k][:, tt, vc * 128:(vc + 1) * 128], AT[k][:, tt * 128:(tt + 1) * 128],
                           True, False, [BB["vt"][k], BB["AT"][k]], [PB[bo[vc]]])
                        for hf in range(2):
                            ci = 2 * tt + hf
                            if ci == 0:
                                if b == 0:
                                    continue
                                st_ap, st_b = Sfin[(it - 1) % 3], BSfin[(it - 1) % 3]
                                lhs = st_ap[:, vc * 128:(vc + 1) * 128]
                            else:
                                st_b = BSall[k][ci]
                                lhs = Sall[k][:, ci, vc * 128:(vc + 1) * 128]
                            mm(ps[bo[vc]][:, ci * 64:(ci + 1) * 64], lhs, qd[k][:, ci * 64:(ci + 1) * 64],
                               False, hf == 1, [st_b, BB["qd"][k]], [PB[bo[vc]]])
                    s4 = (2 * it + vc) % 4
                    S.add("scalar", lambda e, s4=s4, bb=bo[vc]: e.activation(out=sq[s4][:], in_=ps[bb][:], func=AF.Square),
                          reads=[PB[bo[vc]]], writes=[Bsq[s4]])
                bn = bank()
                for vc in range(2):
                    s4 = (2 * it + vc) % 4
                    mm(ps[bn][:], ones_bf[:], sq[s4][:], vc == 0, vc == 1, [Bconst, Bsq[s4]], [PB[bn]])
                kr = nrm_ctr[0] % 2
                nrm_ctr[0] += 1
                S.add("scalar", lambda e, kr=kr, bn=bn: e.activation(out=rs_t[kr][:], in_=ps[bn][:], func=AF.Ln, scale=1.0 / 256, bias=EPS),
                      reads=[PB[bn]], writes=[Brs[kr]])
                S.add("scalar", lambda e, kr=kr: e.activation(out=rstd_t[kr][:], in_=rs_t[kr][:], func=AF.Exp, scale=-0.5), reads=[Brs[kr]], writes=[Brstd[kr]])
                for vc in range(2):
                    gi = GC_GHEAD + l * 2 + vc
                    S.add("vector", lambda e, k=k, vc=vc, kr=kr, gi=gi, bb=bo[vc]: e.scalar_tensor_tensor(
                        out=t0[k][:, vc, :], in0=ps[bb][:], scalar=gcol[:, gi:gi + 1], in1=rstd_t[kr][:], op0=ALU.mult, op1=ALU.mult),
                        reads=[PB[bo[vc]], Brstd[kr], Bconst], writes=[BB["t0"][k]])
                S.add("gpsimd", lambda e, k=k: e.tensor_tensor(out=og[k][:].rearrange("p a b -> p (a b)"), in0=t0[k][:].rearrange("p a b -> p (a b)"),
                                                              in1=sr[k][:].rearrange("p a b -> p (a b)"), op=ALU.mult),
                      reads=[BB["t0"][k], BB["sr"][k]], writes=[BB["og"][k]])

            def out_b(it, h, b):
                k = it % 2
                hs = h % 2
                for c in range(NCK):
                    bw = bank()
                    for vc in range(2):
                        mm(ps[bw][:], wot[hs][:, vc, c * 128:(c + 1) * 128], og[k][:, vc, :], vc == 0, vc == 1,
                           [BB["wot"][hs], BB["og"][k]], [PB[bw]])
                    resid_add(c, b, bw)

            its = [(h, b) for h in range(4) for b in range(NB)]
            n_it = len(its)
            front(0, *its[0])
            if n_it > 1:
                front(1, *its[1])
            mid(0, *its[0])
            for i in range(n_it):
                out(i, *its[i])
                if i + 1 < n_it:
                    mid(i + 1, *its[i + 1])
                out_b(i, *its[i])
                if i + 2 < n_it:
                    front(i + 2, *its[i + 2])

        def kvproj():
            cv = Carver()
            wv = cv.take([128, NCK, 1024], BF16)
            wf = cv.take([128, NCK, 16], BF16)
            wk = [cv.take([128, NCK, 128], BF16) for _ in range(2)]
            kst = [cv.take([128, T], BF16) for _ in range(2)]
            vst = [cv.take([128, 16, 128], BF16) for _ in range(2)]
            lfp = cv.take([128, NT, 16], F32)
            ftmp = [cv.take([128, 16], F32) for _ in range(2)]
            Bwv, Bwf, Blfp = Buf("wv"), Buf("wf"), Buf("lfp")
            Bwk = [Buf("wk0"), Buf("wk1")]
            Bkst = [Buf("kst0"), Buf("kst1")]
            Bvst = [Buf("vst0"), Buf("vst1")]
            Bft = [Buf("ft0"), Buf("ft1")]
            norm(GC_KV)
            S.dma("gpsimd", wv[:], wv_d, Bwv, writes=[Bwv])
            S.dma("gpsimd", wf[:], wf_d, Bwf, writes=[Bwf])
            for i in range(2):
                S.add("gpsimd", lambda e, i=i: e.memset(vst[i][:], 1.0), writes=[Bvst[i]])
            for tt in range(NT):
                k = tt % 2
                tok = slice(tt * 128, (tt + 1) * 128)
                b = tt // 4
                bvs = [bank(), bank()]
                for half in range(2):
                    for c in range(NCK):
                        mm(ps[bvs[half]][:], hnT[:, c, tok], wv[:, c, half * 512:(half + 1) * 512], c == 0, c == NCK - 1,
                           [Bwv, Bhn[c][b]], [PB[bvs[half]]])
                bf_ = bank()
                for c in range(NCK):
                    mm(ps[bf_][:, 0:16], hnT[:, c, tok], wf[:, c, :], c == 0, c == NCK - 1, [Bwf, Bhn[c][b]], [PB[bf_]])
                for half in range(2):
                    src = ps[bvs[half]][:].rearrange("p (a two d) -> p a two d", two=2, d=64)
                    dst = vst[k][:, half * 8:(half + 1) * 8, :].rearrange("p (a two) d -> p a two d", two=2)
                    S.add("scalar", lambda e, src=src, dst=dst: e.copy(out=dst[:, :, 0, 0:64], in_=src[:, :, 0, :]),
                          reads=[PB[bvs[half]]], writes=[Bvst[k]])
                    S.add("vector", lambda e, src=src, dst=dst: e.tensor_copy(out=dst[:, :, 1, 64:128], in_=src[:, :, 1, :]),
                          reads=[PB[bvs[half]]], writes=[Bvst[k]])
                S.dma("sync", v_d[tok, :], vst[k][:].rearrange("p a b -> p (a b)"), Bvst[k], reads=[Bvst[k]], writes=[Bv_d])
                S.add("vector", lambda e, k=k, bf_=bf_: e.tensor_tensor(out=ftmp[k][:], in0=ps[bf_][:, 0:16], in1=bfb[:], op=ALU.add),
                      reads=[PB[bf_], Bconst], writes=[Bft[k]])
                S.add("scalar", lambda e, k=k: e.activation(out=ftmp[k][:], in_=ftmp[k][:], func=AF.Exp, scale=-1.0),
                      reads=[Bft[k]], writes=[Bft[k]])
                S.add("scalar", lambda e, k=k, tt=tt: e.activation(out=lfp[:, tt, :], in_=ftmp[k][:], func=AF.Ln, bias=1.0),
                      reads=[Bft[k]], writes=[Blfp])
            for tt in range(NT):
                bc = bank()
                mm(ps[bc][:, 0:16], U32[:], lfp[:, tt, :], True, tt == 0, [Bconst, Blfp], [PB[bc]])
                for t2 in range(tt):
                    mm(ps[bc][:, 0:16], ones32[:], lfp[:, t2, :], False, t2 == tt - 1, [Bconst, Blfp], [PB[bc]])
                S.add("vector", lambda e, bc=bc, tt=tt: e.tensor_copy(out=ctok[:, tt, :], in_=ps[bc][:, 0:16]), reads=[PB[bc]], writes=[Bctok])
            for qb in range(NB):
                bc = bank()
                mm(ps[bc][:, 0:16], E032[:], ctok[:, 4 * qb, :], True, True, [Bconst, Bctok], [PB[bc]])
                S.add("vector", lambda e, bc=bc, qb=qb: e.tensor_copy(out=cref[:, qb, :], in_=ps[bc][:, 0:16]), reads=[PB[bc]], writes=[Bcref])
                for kt in range(4 * qb + 4):
                    S.add("vector", lambda e, qb=qb, kt=kt: e.tensor_tensor(out=bias_t[:, kt, qb, :], in0=ctok[:, kt, :], in1=cref[:, qb, :], op=ALU.subtract),
                          reads=[Bctok, Bcref], writes=[Bbias])
            for hp in range(8):
                k = hp % 2
                S.dma("gpsimd", wk[k][:], wk_d[hp], Bwk[k], writes=[Bwk[k]])
                for b in range(NB):
                    bk = bank()
                    for c in range(NCK):
                        mm(ps[bk][:], wk[k][:, c, :], hnT[:, c, tbs(b)], c == 0, c == NCK - 1, [Bwk[k], Bhn[c][b]], [PB[bk]])
                    S.add("scalar", lambda e, k=k, bk=bk, b=b: e.copy(out=kst[k][:, tbs(b)], in_=ps[bk][:]), reads=[PB[bk]], writes=[Bkst[k]])
                S.dma("sync", kT_d[hp], kst[k][:], Bkst[k], reads=[Bkst[k]], writes=[Bk_d[hp]])

        def fox(l):
            j = l - 2
            cv = Carver()
            attn = cv.take([128, 8, T], BF16)
            cv.off = (cv.off + 31) // 32 * 32
            off_slot0 = cv.off
            QT, KT, Vh = [], [], []
            for _k in range(2):
                QT.append(cv.take([128, 2, T], BF16))
                KT.append(cv.take([128, T], BF16))
                Vh.append(cv.take([128, NT, 256], BF16))
                if _k == 0:
                    slot0_bytes = cv.off - off_slot0
            cvw = Carver()
            cvw.off = off_slot0
            wo = cvw.take([128, NCK, 1024], BF16)
            assert cvw.off - off_slot0 <= slot0_bytes, "wo must fit in the slot-0 group"
            Bwo = Buf("wo")
            wq = [cv.take([128, NCK, 128], BF16) for _ in range(2)]
            PT = [cv.take([128, 512], BF16) for _ in range(4)]
            rden = [cv.take([128, 512], F32) for _ in range(2)]
            BQT = [Buf("QT0"), Buf("QT1")]
            BKT = [Buf("KT0"), Buf("KT1")]
            BVh = [Buf("Vh0"), Buf("Vh1")]
            Bwq = [Buf("wq0"), Buf("wq1")]
            BPT = [Buf("PT%d" % i) for i in range(4)]
            Brden = [Buf("rden0"), Buf("rden1")]
            Battn = [[Buf("attn") for _ in range(NB)] for _ in range(8)]
            for k in range(2):
                S.add("gpsimd", lambda e, k=k: e.memset(QT[k][64:128, 0, :], 0.0), writes=[BQT[k]])
                S.add("gpsimd", lambda e, k=k: e.memset(QT[k][0:64, 1, :], 0.0), writes=[BQT[k]])
            norm(GC_ATTN + l * 8)
            pctr = 0
            qctr = 0
            def qproj(hp):
                k = hp % 2
                S.dma("gpsimd", wq[k][:], wq_d[j, hp], Bwq[k], writes=[Bwq[k]])
                S.dma("sync", KT[k][:], kT_d[hp], BKT[k], reads=[Bk_d[hp]], writes=[BKT[k]])
                S.dma("sync", Vh[k][:], v_d[:, hp * 256:(hp + 1) * 256].rearrange("(t p) c -> p t c", p=128), BVh[k],
                      reads=[Bv_d], writes=[BVh[k]])
                for b in range(NB):
                    bk = bank(4, 8)
                    for c in range(NCK):
                        mm(ps[bk][:], wq[k][:, c, :], hnT[:, c, tbs(b)], c == 0, c == NCK - 1, [Bwq[k], Bhn[c][b]], [PB[bk]])
                    for hh in range(2):
                        rows = slice(64 * hh, 64 * hh + 64)
                        S.add("scalar", lambda e, k=k, bk=bk, b=b, hh=hh, rows=rows: e.mul(out=QT[k][rows, hh, tbs(b)], in_=ps[bk][rows, :], mul=0.125),
                              reads=[PB[bk]], writes=[BQT[k]])

            qproj(0)
            for hp in range(8):
                k = hp % 2
                for qb in range(NB):
                    if qb == min(1, NB - 1) and hp == 7:
                        S.dma("gpsimd", wo[:], wo_d[j], Bwo, writes=[Bwo, BQT[0], BKT[0], BVh[0]])
                    if qb == min(1, NB - 1) and hp + 1 < 8:
                        qproj(hp + 1)
                    nk = 4 * qb + 4
                    ob = [2 * (qctr % 2), 2 * (qctr % 2) + 1]
                    qctr += 1
                    iters = [(kt, hh) for kt in range(nk) for hh in range(2)]
                    sbank = {}

                    def qk(i):
                        kt, hh = iters[i]
                        off = max(0, kt - 4 * qb) * 128
                        bs = bank(4, 8)
                        sbank[i] = bs
                        mm(ps[bs][:, off:512], KT[k][:, kt * 128:(kt + 1) * 128], QT[k][:, hh, qb * 512 + off:(qb + 1) * 512], True, True,
                           [BKT[k], BQT[k]], [PB[bs]])

                    def rest(i, pctr):
                        kt, hh = iters[i]
                        off = max(0, kt - 4 * qb) * 128
                        bs = sbank[i]
                        p = pctr % 4
                        hd = hp * 2 + hh
                        S.add("scalar", lambda e, p=p, bs=bs, off=off, kt=kt, qb=qb, hd=hd: e.activation(
                            out=PT[p][:, off:512], in_=ps[bs][:, off:512], func=AF.Exp, bias=bias_t[:, kt, qb, hd:hd + 1]),
                            reads=[PB[bs], Bbias], writes=[BPT[p]])
                        if kt >= 4 * qb:
                            S.add("gpsimd", lambda e, p=p, off=off: e.tensor_tensor(out=PT[p][:, off:off + 128], in0=PT[p][:, off:off + 128],
                                                                                  in1=tri[:], op=ALU.mult),
                                  reads=[BPT[p], Bconst], writes=[BPT[p]])
                        mm(ps[ob[hh]][:, off:512], Vh[k][:, kt, hh * 128:(hh + 1) * 128], PT[p][:, off:512], kt == 0, kt == nk - 1,
                           [BVh[k], BPT[p]], [PB[ob[hh]]])

                    LOOK = 2
                    for i in range(min(LOOK, len(iters))):
                        qk(i)
                    for i in range(len(iters)):
                        if i + LOOK < len(iters):
                            qk(i + LOOK)
                        rest(i, pctr)
                        pctr += 1
                    for hh in range(2):
                        num = slice(64 * hh, 64 * hh + 64)
                        den = slice(64 * (1 - hh), 64 * (1 - hh) + 64)
                        S.add("vector", lambda e, hh=hh, num=num, den=den, obk=ob[hh]: e.reciprocal(out=rden[hh][num, :], in_=ps[obk][den, :]),
                              reads=[PB[ob[hh]]], writes=[Brden[hh]])
                        S.add("vector", lambda e, hh=hh, num=num, hp=hp, qb=qb, obk=ob[hh]: e.tensor_tensor(out=attn[num, hp, tbs(qb)], in0=ps[obk][num, :],
                                                                                                          in1=rden[hh][num, :], op=ALU.mult),
                              reads=[PB[ob[hh]], Brden[hh]], writes=[Battn[hp][qb]])
            for c in range(NCK):
                for b in range(NB):
                    bk = bank()
                    for fc in range(8):
                        mm(ps[bk][:], wo[:, fc, c * 128:(c + 1) * 128], attn[:, fc, tbs(b)], fc == 0, fc == 7, [Bwo, Battn[fc][b]], [PB[bk]])
                    resid_add(c, b, bk)

        def final(s):
            cv = Carver()
            ystage = [cv.take([128, 512], F32) for _ in range(4)]
            Bys = [Buf("ys%d" % i) for i in range(4)]
            norm(GC_FINAL, final_seq=s, ystage=ystage, Bystage=Bys)

        consts()
        for s in range(NSEQ):
            for c in range(NCK):
                S.dma("sync", hT[:, c, :], xT_d[s, :, c, :], Bh[c][0], writes=Bh[c])
            for l in range(n_gla):
                S.barrier()
                gla(l)
                S.barrier()
                ffn(l)
            if n_fox > 0:
                S.barrier()
                kvproj()
            for l in range(2, 2 + n_fox):
                S.barrier()
                fox(l)
                S.barrier()
                ffn(l)
            S.barrier()
            if do_final:
                final(s)
            S.barrier()
        S.emit(nc, st)
    return nc, S


def prep_weights(inp):
    f = lambda a: np.ascontiguousarray(a, dtype=np.float32)
    col = lambda v: np.asarray(v, np.float32).reshape(-1, 128).T
    gcol = np.concatenate([
        col(inp["attn_norm"]), col(inp["ffn_norm"]), col(inp["kv_norm"]), col(inp["final_norm"]),
        col(inp["gla_b_alpha"]), col(inp["gla_g_head"])], axis=1)
    assert gcol.shape == (128, NGC)
    w_in = np.asarray(inp["gla_w_in"], np.float32)
    wr = w_in.reshape(2, NCK, 128, 3088)
    q = wr[..., 0:512].reshape(2, NCK, 128, 4, 128)
    k = wr[..., 512:1024].reshape(2, NCK, 128, 4, 128)
    v = wr[..., 1024:2048].reshape(2, NCK, 128, 4, 256)
    r = wr[..., 2048:3072].reshape(2, NCK, 128, 4, 256)
    slab = np.concatenate([q, k, v, r], axis=-1)
    wgla = slab.transpose(0, 3, 2, 1, 4)
    wa = wr[..., 3072:3088].transpose(0, 2, 1, 3)
    wout = np.asarray(inp["gla_w_out"], np.float32).reshape(2, 4, 2, 128, 1024).transpose(0, 1, 3, 2, 4)
    gu = np.asarray(inp["ffn_w_gu"], np.float32).reshape(4, NCK, 128, 2, NSLAB, 2, 128)
    wgu = gu.transpose(0, 4, 2, 1, 3, 5, 6).reshape(4, NSLAB, 128, NCK, 512)
    wd = np.asarray(inp["ffn_w_down"], np.float32).reshape(4, NJ, 128, 1024).transpose(0, 2, 1, 3)
    wkv = np.asarray(inp["w_kv"], np.float32).reshape(NCK, 128, 2064)
    wk = wkv[..., 0:1024].reshape(NCK, 128, 8, 128).transpose(2, 1, 0, 3)
    wv = wkv[..., 1024:2048].transpose(1, 0, 2)
    wf = wkv[..., 2048:2064].transpose(1, 0, 2)
    wq = np.asarray(inp["fox_w_q"], np.float32).reshape(2, NCK, 128, 8, 128).transpose(0, 3, 2, 1, 4)
    wo = np.asarray(inp["fox_w_o"], np.float32).reshape(2, NCK, 128, 1024).transpose(0, 2, 1, 3)
    bfb = np.tile(np.asarray(inp["b_f"], np.float32)[None, :], (128, 1))
    return {"gcol": f(gcol), "bfb": f(bfb), "wgla": f(wgla), "wout": f(wout), "wa": f(wa),
            "wup": f(inp["gla_w_alpha_up"]), "wgu": f(wgu), "wd": f(wd), "wk": f(wk), "wv": f(wv),
            "wf": f(wf), "wq": f(wq), "wo": f(wo)}


def prep_x(xs):
    n, T, _ = xs.shape
    return np.ascontiguousarray(xs.reshape(n, T, NCK, 128).transpose(0, 3, 2, 1), dtype=np.float32)


def unprep_y(yT):
    n, _, _, T = yT.shape
    return np.ascontiguousarray(yT.transpose(0, 3, 2, 1).reshape(n, T, D))


_NC_CACHE = {}
N_CORES = 8


def kernel(**inputs):
    x = np.asarray(inputs["x"], np.float32)
    B, T, _ = x.shape
    n_cores = N_CORES
    per = B // n_cores
    key = (T, per)
    if key not in _NC_CACHE:
        _NC_CACHE[key] = build_program(T=T, NSEQ=per)[0]
    nc = _NC_CACHE[key]
    w = prep_weights(inputs)
    in_maps = []
    for c in range(n_cores):
        m = dict(w)
        m["xT"] = prep_x(x[c * per:(c + 1) * per])
        in_maps.append(m)
    res = run_bass_kernel_spmd(nc, in_maps, core_ids=list(range(n_cores)))
    out = np.concatenate([unprep_y(r["yT"]) for r in res.results], axis=0)
    return out.astype(np.float32)
```

```python
import numpy as np
from contextlib import ExitStack
import concourse.bass as bass
import concourse.mybir as mybir
from concourse.bass_utils import run_bass_kernel_spmd

F32 = mybir.dt.float32
BF16 = mybir.dt.bfloat16
AF = mybir.ActivationFunctionType
ALU = mybir.AluOpType
AX = mybir.AxisListType

ENGS = ["sync", "scalar", "vector", "gpsimd", "tensor"]


class Buf:
    __slots__ = ("name", "w", "rs", "dsem")

    def __init__(self, name):
        self.name = name
        self.w = None
        self.rs = {}
        self.dsem = None


class Op:
    __slots__ = ("eng", "fn", "deps", "observed", "count", "is_dma", "dsem")


class Sched:
    def __init__(self):
        self.ops = {e: [] for e in ENGS}
        self.n_dsem = 0
        self.last_on_dsem = {}
        self.dma_ops = []
        self.dsem_by_name = {}
        self.nops = 0

    def add(self, eng, fn, reads=(), writes=(), dma=False, dsem=None):
        op = Op()
        op.eng = eng
        op.fn = fn
        op.is_dma = dma
        op.dsem = dsem
        op.observed = False
        op.count = None
        deps = {}
        for b in reads:
            if b.w is not None:
                deps[id(b.w)] = b.w
        for b in writes:
            if b.w is not None:
                deps[id(b.w)] = b.w
            for r in b.rs.values():
                deps[id(r)] = r
        if dma:
            prev = self.last_on_dsem.get(dsem)
            if prev is not None:
                deps[id(prev)] = prev
            self.last_on_dsem[dsem] = op
            self.dma_ops.append(op)
        dl = []
        for d in deps.values():
            if d is op:
                continue
            if eng == "tensor" and d.eng == "tensor" and not d.is_dma and not dma:
                continue
            d.observed = True
            dl.append(d)
        op.deps = dl
        key = ("d", dsem) if dma else ("e", eng)
        for b in reads:
            b.rs[key] = op
        for b in writes:
            b.w = op
            b.rs = {}
        self.ops[eng].append(op)
        self.nops += 1
        return op

    def new_dsem(self):
        self.n_dsem += 1
        return self.n_dsem - 1

    def dma(self, eng, out_ap, in_ap, sb, reads=(), writes=()):
        if sb.dsem is None:
            if sb.name not in self.dsem_by_name:
                self.dsem_by_name[sb.name] = self.new_dsem()
            sb.dsem = self.dsem_by_name[sb.name]
        return self.add(eng, lambda e, o=out_ap, i=in_ap: e.dma_start(out=o, in_=i),
                        reads=reads, writes=writes, dma=True, dsem=sb.dsem)

    def emit(self, nc, stack):
        esem = {e: stack.enter_context(nc.semaphore("pg_" + e)) for e in ENGS}
        dsem = [stack.enter_context(nc.semaphore("dm_%d" % i)) for i in range(self.n_dsem)]
        for e in ENGS:
            c = 0
            for op in self.ops[e]:
                if not op.is_dma and op.observed:
                    c += 1
                    op.count = c
        dc = [0] * self.n_dsem
        for op in self.dma_ops:
            dc[op.dsem] += 16
            op.count = dc[op.dsem]
        block = stack.enter_context(nc.Block())

        def run(e_name):
            def body(eng):
                waited = {}
                for op in self.ops[e_name]:
                    need = {}
                    for d in op.deps:
                        k = ("d", d.dsem) if d.is_dma else ("e", d.eng)
                        if need.get(k, 0) < d.count:
                            need[k] = d.count
                    for k, v in need.items():
                        if waited.get(k, 0) >= v:
                            continue
                        waited[k] = v
                        s = dsem[k[1]] if k[0] == "d" else esem[k[1]]
                        eng.wait_ge(s, v)
                    if op.fn is None:
                        continue
                    ins = op.fn(eng)
                    if op.is_dma:
                        ins.then_inc(dsem[op.dsem], 16)
                    elif op.observed:
                        ins.then_inc(esem[e_name], 1)
                if e_name == "sync":
                    for i in range(self.n_dsem):
                        if dc[i] > 0:
                            eng.wait_ge(dsem[i], dc[i])
            return body

        block.sync(run("sync"))
        block.scalar(run("scalar"))
        block.vector(run("vector"))
        block.gpsimd(run("gpsimd"))
        block.tensor(run("tensor"))

    def barrier(self):
        lasts = []
        for e in ENGS:
            for op in reversed(self.ops[e]):
                if not op.is_dma and op.fn is not None:
                    lasts.append(op)
                    break
        lasts.extend(self.last_on_dsem.values())
        for e in ENGS:
            op = Op()
            op.eng = e
            op.fn = None
            op.is_dma = False
            op.dsem = None
            op.observed = False
            op.count = None
            op.deps = list(lasts)
            for d in op.deps:
                d.observed = True
            self.ops[e].append(op)


D = 1024
NCK = 8
DFF = 2816
NJ = 22
NSLAB = 11
GROUPS = [(0, 3), (3, 6), (6, 9), (9, 11)]
EPS = 1e-6
GLA_SCALE = 128 ** -0.5

GC_ATTN = 0
GC_FFN = 32
GC_KV = 64
GC_FINAL = 72
GC_BALPHA = 80
GC_GHEAD = 88
NGC = 92


def build_program(T=2048, NSEQ=2, n_gla=2, n_fox=2, do_final=True):
    NB = T // 512
    NT = T // 128
    nc = bass.Bass("TRN2", target_bir_lowering=False)
    S = Sched()
    dt = lambda name, shape, dty, kind: nc.dram_tensor(name, shape, dty, kind=kind).ap()
    xT_d = dt("xT", [NSEQ, 128, NCK, T], F32, "ExternalInput")
    gcol_d = dt("gcol", [128, NGC], F32, "ExternalInput")
    bfb_d = dt("bfb", [128, 16], F32, "ExternalInput")
    wgla_d = dt("wgla", [2, 4, 128, NCK, 768], F32, "ExternalInput")
    wout_d = dt("wout", [2, 4, 128, 2, 1024], F32, "ExternalInput")
    wa_d = dt("wa", [2, 128, NCK, 16], F32, "ExternalInput")
    wup_d = dt("wup", [2, 16, 512], F32, "ExternalInput")
    wgu_d = dt("wgu", [4, NSLAB, 128, NCK, 512], F32, "ExternalInput")
    wd_d = dt("wd", [4, 128, NJ, 1024], F32, "ExternalInput")
    wk_d = dt("wk", [8, 128, NCK, 128], F32, "ExternalInput")
    wv_d = dt("wv", [128, NCK, 1024], F32, "ExternalInput")
    wf_d = dt("wf", [128, NCK, 16], F32, "ExternalInput")
    wq_d = dt("wq", [2, 8, 128, NCK, 128], F32, "ExternalInput")
    wo_d = dt("wo", [2, 128, NCK, 1024], F32, "ExternalInput")
    yT_d = dt("yT", [NSEQ, 128, NCK, T], F32, "ExternalOutput")
    kT_d = dt("kT_s", [8, 128, T], BF16, "Internal")
    v_d = dt("v_s", [T, 2048], BF16, "Internal")
    Bk_d = [Buf("kTd%d" % i) for i in range(8)]
    Bv_d = Buf("vd")

    st = ExitStack()
    with st:
        sb = lambda name, shape, dty: st.enter_context(nc.sbuf_tensor(name, shape, dty))
        hT = sb("hT", [128, NCK, T], F32)
        hnT = sb("hnT", [128, NCK, T], BF16)
        ones_bf = sb("ones_bf", [128, 128], BF16)
        ident_bf = sb("ident_bf", [128, 128], BF16)
        maskbd = sb("maskbd", [128, 512], BF16)
        tri = sb("tri", [128, 128], BF16)
        scanmask = sb("scanmask", [128, 512], F32)
        U32 = sb("U32", [128, 128], F32)
        ones32 = sb("ones32", [128, 128], F32)
        E032 = sb("E032", [128, 128], F32)
        gcol = sb("gcol_s", [128, NGC], F32)
        nbal = sb("nbal", [128, 8], F32)
        bfb = sb("bfb_s", [128, 16], F32)
        sq = [sb("sq%d" % i, [128, 512], BF16) for i in range(4)]
        rs_t = [sb("rs%d" % i, [128, 512], F32) for i in range(2)]
        rstd_t = [sb("rstd%d" % i, [128, 512], F32) for i in range(2)]
        ctok = sb("ctok", [128, NT, 16], F32)
        cref = sb("cref", [128, NB, 16], F32)
        bias_t = sb("bias_t", [128, NT, NB, 16], F32)
        RB = 86 * 1024
        R = sb("R", [128, RB], mybir.dt.uint8)
        ps = [st.enter_context(nc.psum_tensor("ps%d" % i, [128, 512], F32)) for i in range(8)]
        PB = [Buf("ps%d" % i) for i in range(8)]

        class Carver:
            def __init__(self):
                self.off = 0

            def take(self, shape, dty):
                esz = 2 if dty == BF16 else 4
                n = int(np.prod(shape[1:])) * esz
                o = (self.off + 31) // 32 * 32
                assert o + n <= RB, ("region overflow", o + n, RB)
                self.off = o + n
                v = R[0:shape[0], o:o + n].bitcast(dty)
                if len(shape) == 3:
                    v = v.rearrange("p (a b) -> p a b", b=shape[2])
                elif len(shape) == 4:
                    v = v.rearrange("p (a b c) -> p a b c", b=shape[2], c=shape[3])
                return v

        bank_rr = [0]

        def bank(lo=0, hi=8):
            i = bank_rr[0] % (hi - lo) + lo
            bank_rr[0] += 1
            return i

        def mm(out, lhsT, rhs, start, stop, reads, writes):
            S.add("tensor", lambda e: e.matmul(out, lhsT, rhs, start=start, stop=stop),
                  reads=reads, writes=writes)

        Bh = [[Buf("h%d_%d" % (c, b)) for b in range(NB)] for c in range(NCK)]
        Bhn = [[Buf("hn%d_%d" % (c, b)) for b in range(NB)] for c in range(NCK)]
        Bconst = Buf("const")
        Bsq = [Buf("sq%d" % i) for i in range(4)]
        Brs = [Buf("rs%d" % i) for i in range(2)]
        Brstd = [Buf("rstd%d" % i) for i in range(2)]
        Bctok = Buf("ctok")
        Bcref = Buf("cref")
        Bbias = Buf("bias")
        tbs = lambda b: slice(b * 512, (b + 1) * 512)

        def consts():
            G = "gpsimd"
            S.dma("sync", gcol[:], gcol_d, Bconst, writes=[Bconst])
            S.dma("sync", bfb[:], bfb_d, Bconst, writes=[Bconst])
            S.add(G, lambda e: e.memset(ones_bf[:], 1.0), writes=[Bconst])
            S.add(G, lambda e: e.memset(ones32[:], 1.0), writes=[Bconst])
            S.add(G, lambda e: e.affine_select(out=ident_bf[:], in_=ones_bf[:], pattern=[[-1, 128]],
                                               compare_op=ALU.is_equal, fill=0.0, base=0,
                                               channel_multiplier=1), reads=[Bconst], writes=[Bconst])
            S.add(G, lambda e: e.affine_select(out=tri[:], in_=ones_bf[:], pattern=[[1, 128]],
                                               compare_op=ALU.is_ge, fill=0.0, base=0,
                                               channel_multiplier=-1), reads=[Bconst], writes=[Bconst])
            S.add(G, lambda e: e.affine_select(out=U32[:], in_=ones32[:], pattern=[[1, 128]],
                                               compare_op=ALU.is_ge, fill=0.0, base=0,
                                               channel_multiplier=-1), reads=[Bconst], writes=[Bconst])
            S.add(G, lambda e: e.affine_select(out=E032[:], in_=ones32[:], pattern=[[0, 128]],
                                               compare_op=ALU.is_ge, fill=0.0, base=0,
                                               channel_multiplier=-1), reads=[Bconst], writes=[Bconst])
            for r in range(4):
                S.add(G, lambda e, r=r: e.tensor_copy(out=maskbd[:, r * 128:(r + 1) * 128], in_=tri[:]),
                      reads=[Bconst], writes=[Bconst])
                S.add(G, lambda e, r=r: e.memset(maskbd[0:64, r * 128 + 64:(r + 1) * 128], 0.0),
                      writes=[Bconst])
            S.add(G, lambda e: e.memset(scanmask[:], 1.0), writes=[Bconst])
            S.add(G, lambda e: e.memset(scanmask[:].rearrange("p (c k) -> p c k", k=64)[:, :, 0:1], 0.0),
                  writes=[Bconst])
            S.add(G, lambda e: e.tensor_scalar(out=nbal[:], in0=gcol[:, GC_BALPHA:GC_BALPHA + 8], scalar1=-1.0,
                                               scalar2=None, op0=ALU.mult), reads=[Bconst], writes=[Bconst])

        nrm_ctr = [0]

        def norm(gbase, final_seq=None, ystage=None, Bystage=None):
            for b in range(NB):
                k = nrm_ctr[0] % 2
                nrm_ctr[0] += 1
                bk = bank()
                for c in range(NCK):
                    s4 = (b * NCK + c) % 4
                    S.add("scalar", lambda e, c=c, s4=s4, b=b: e.activation(out=sq[s4][:], in_=hT[:, c, tbs(b)], func=AF.Square),
                          reads=[Bh[c][b]], writes=[Bsq[s4]])
                    mm(ps[bk][:], ones_bf[:], sq[s4][:], c == 0, c == NCK - 1, [Bconst, Bsq[s4]], [PB[bk]])
                S.add("scalar", lambda e, k=k, bk=bk: e.activation(out=rs_t[k][:], in_=ps[bk][:], func=AF.Ln, scale=1.0 / D, bias=EPS),
                      reads=[PB[bk]], writes=[Brs[k]])
                S.add("scalar", lambda e, k=k: e.activation(out=rstd_t[k][:], in_=rs_t[k][:], func=AF.Exp, scale=-0.5), reads=[Brs[k]], writes=[Brstd[k]])
                for c in range(NCK):
                    if final_seq is None:
                        eng = "vector"
                        S.add(eng, lambda e, c=c, k=k, b=b: e.scalar_tensor_tensor(
                            out=hnT[:, c, tbs(b)], in0=hT[:, c, tbs(b)], scalar=gcol[:, gbase + c:gbase + c + 1],
                            in1=rstd_t[k][:], op0=ALU.mult, op1=ALU.mult),
                            reads=[Bh[c][b], Brstd[k], Bconst], writes=[Bhn[c][b]])
                    else:
                        ys = (b * NCK + c) % len(ystage)
                        S.add("vector", lambda e, c=c, k=k, b=b, ys=ys: e.scalar_tensor_tensor(
                            out=ystage[ys][:], in0=hT[:, c, tbs(b)], scalar=gcol[:, gbase + c:gbase + c + 1],
                            in1=rstd_t[k][:], op0=ALU.mult, op1=ALU.mult),
                            reads=[Bh[c][b], Brstd[k], Bconst], writes=[Bystage[ys]])
                        S.dma("sync", yT_d[final_seq, :, c, tbs(b)], ystage[ys][:], Bystage[ys], reads=[Bystage[ys]])

        def resid_add(c, b, bk):
            S.add("vector", lambda e: e.tensor_tensor(out=hT[:, c, tbs(b)], in0=hT[:, c, tbs(b)], in1=ps[bk][:], op=ALU.add),
                  reads=[Bh[c][b], PB[bk]], writes=[Bh[c][b]])

        def ffn(l):
            cv = Carver()
            act = cv.take([128, 6, T], BF16)
            wgu = [cv.take([128, NCK, 512], BF16) for _ in range(3)]
            wdn = [cv.take([128, 6, 1024], BF16) for _ in range(2)]
            sg = [cv.take([128, 512], BF16) for _ in range(2)]
            ub = [cv.take([128, 512], BF16) for _ in range(2)]
            Bact = [[Buf("act") for _ in range(NB)] for _ in range(6)]
            Bwgu = [Buf("wgu%d" % i) for i in range(3)]
            Bwdn = [Buf("wdn%d" % i) for i in range(2)]
            Bsg = [Buf("sg"), Buf("sg")]
            Bub = [Buf("ub"), Buf("ub")]
            norm(GC_FFN + l * 8)
            ectr = 0
            for gi, (s0, s1) in enumerate(GROUPS):
                nj = (s1 - s0) * 2
                S.dma("gpsimd", wdn[gi % 2][:, 0:nj, :], wd_d[l, :, 2 * s0:2 * s1, :], Bwdn[gi % 2], writes=[Bwdn[gi % 2]])
                for sl in range(s0, s1):
                    w = sl % 3
                    S.dma("gpsimd", wgu[w][:], wgu_d[l, sl], Bwgu[w], writes=[Bwgu[w]])
                    for jj in range(2):
                        jl = (sl - s0) * 2 + jj
                        for b in range(NB):
                            bg, bu = bank(), bank()
                            for c in range(NCK):
                                mm(ps[bg][:], wgu[w][:, c, jj * 128:(jj + 1) * 128], hnT[:, c, tbs(b)], c == 0, c == NCK - 1,
                                   [Bwgu[w], Bhn[c][b]], [PB[bg]])
                            for c in range(NCK):
                                mm(ps[bu][:], wgu[w][:, c, 256 + jj * 128:256 + (jj + 1) * 128], hnT[:, c, tbs(b)], c == 0, c == NCK - 1,
                                   [Bwgu[w], Bhn[c][b]], [PB[bu]])
                            k = ectr % 2
                            ectr += 1
                            S.add("scalar", lambda e, k=k, bg=bg: e.activation(out=sg[k][:], in_=ps[bg][:], func=AF.Silu),
                                  reads=[PB[bg]], writes=[Bsg[k]])
                            S.add("vector", lambda e, k=k, bu=bu, jl=jl, b=b: e.tensor_tensor(out=act[:, jl, tbs(b)], in0=ps[bu][:], in1=sg[k][:], op=ALU.mult),
                                  reads=[PB[bu], Bsg[k]], writes=[Bact[jl][b]])
                for c in range(NCK):
                    for b in range(NB):
                        bk = bank()
                        for jl in range(nj):
                            mm(ps[bk][:], wdn[gi % 2][:, jl, c * 128:(c + 1) * 128], act[:, jl, tbs(b)], jl == 0, jl == nj - 1,
                               [Bwdn[gi % 2], Bact[jl][b]], [PB[bk]])
                        resid_add(c, b, bk)

        def gla(l):
            cv = Carver()
            wsl = [cv.take([128, NCK, 768], BF16) for _ in range(2)]
            wot = [cv.take([128, 2, 1024], BF16) for _ in range(2)]
            wa = cv.take([128, NCK, 16], BF16)
            wup = cv.take([16, 512], BF16)
            alow = cv.take([16, T], BF16)
            Sf = [cv.take([128, 256], F32) for _ in range(2)]
            dec = [cv.take([128, 8], F32) for _ in range(2)]
            P2 = lambda shape, dty: [cv.take(shape, dty) for _ in range(2)]
            lp1 = cv.take([128, 512], F32)
            lp = [lp1, lp1]
            cs1 = cv.take([128, 512], F32)
            cs = [cs1, cs1]
            eb = P2([128, 512], BF16)
            enb = P2([128, 512], BF16)
            qd = P2([128, 512], BF16)
            kd = P2([128, 512], BF16)
            kk = P2([128, 512], BF16)
            kktok = P2([128, 4, 128], BF16)
            vt = P2([128, 4, 256], BF16)
            AT = P2([128, 512], BF16)
            Sall = P2([128, 8, 256], BF16)
            Sfin = [cv.take([128, 256], BF16) for _ in range(3)]
            sr = P2([128, 2, 512], BF16)
            og = P2([128, 2, 512], BF16)
            t0 = og
            names = "wsl wot lp cs eb enb qd kd kk kktok vt AT sr og dec".split()
            BB = {n: [Buf(n + "0"), Buf(n + "1")] for n in names}
            BB["lp"][1] = BB["lp"][0]
            BB["cs"][1] = BB["cs"][0]
            BB["t0"] = BB["og"]
            Bwa, Bwup, Balow = Buf("wa"), Buf("wup"), Buf("alow")
            BSf = [Buf("Sf0"), Buf("Sf1")]
            BSall = [[Buf("Sall") for _ in range(8)] for _ in range(2)]
            BSfin = [Buf("Sfin") for _ in range(3)]
            norm(GC_ATTN + l * 8)
            S.dma("gpsimd", wa[:], wa_d[l], Bwa, writes=[Bwa])
            S.dma("gpsimd", wup[:], wup_d[l], Bwup, writes=[Bwup])
            for b in range(NB):
                bk = bank()
                for c in range(NCK):
                    mm(ps[bk][0:16, :], wa[:, c, :], hnT[:, c, tbs(b)], c == 0, c == NCK - 1, [Bwa, Bhn[c][b]], [PB[bk]])
                S.add("scalar", lambda e, bk=bk, b=b: e.copy(out=alow[:, tbs(b)], in_=ps[bk][0:16, :]), reads=[PB[bk]], writes=[Balow])
            sfi = [0]

            def front(it, h, b):
                k = it % 2
                hs = h % 2
                if b == 0:
                    S.dma("gpsimd", wsl[hs][:], wgla_d[l, h], BB["wsl"][hs], writes=[BB["wsl"][hs]])
                    S.dma("gpsimd", wot[hs][:], wout_d[l, h], BB["wot"][hs], writes=[BB["wot"][hs]])
                W = wsl[hs]
                BW = BB["wsl"][hs]
                rd_hn = [Bhn[c][b] for c in range(NCK)]
                bx = bank()
                mm(ps[bx][:], wup[:, h * 128:(h + 1) * 128], alow[:, tbs(b)], True, True, [Bwup, Balow], [PB[bx]])
                S.add("scalar", lambda e, k=k, bx=bx, h=h: e.activation(out=lp[k][:], in_=ps[bx][:], func=AF.Exp, scale=-1.0,
                                                                     bias=nbal[:, l * 4 + h:l * 4 + h + 1]),
                      reads=[PB[bx], Bconst], writes=[BB["lp"][k]])
                S.add("scalar", lambda e, k=k: e.activation(out=lp[k][:], in_=lp[k][:], func=AF.Ln, bias=1.0),
                      reads=[BB["lp"][k]], writes=[BB["lp"][k]])
                S.add("vector", lambda e, k=k: e.tensor_tensor_scan(out=cs[k][:], data0=scanmask[:], data1=lp[k][:], initial=0.0,
                                                                   op0=ALU.mult, op1=ALU.add),
                      reads=[BB["lp"][k], Bconst], writes=[BB["cs"][k]])
                S.add("scalar", lambda e, k=k: e.activation(out=eb[k][:], in_=cs[k][:], func=AF.Exp, scale=-1.0 / 16),
                      reads=[BB["cs"][k]], writes=[BB["eb"][k]])
                S.add("scalar", lambda e, k=k: e.activation(out=enb[k][:], in_=cs[k][:], func=AF.Exp, scale=1.0 / 16),
                      reads=[BB["cs"][k]], writes=[BB["enb"][k]])
                S.add("scalar", lambda e, k=k: e.activation(out=dec[k][:], in_=cs[k][:].rearrange("p (c k) -> p c k", k=64)[:, :, 63],
                                                            func=AF.Exp, scale=-1.0 / 16),
                      reads=[BB["cs"][k]], writes=[BB["dec"][k]])
                bq, bkk = bank(), bank()
                for c in range(NCK):
                    mm(ps[bq][:], W[:, c, 0:128], hnT[:, c, tbs(b)], c == 0, c == NCK - 1, [BW, rd_hn[c]], [PB[bq]])
                for c in range(NCK):
                    mm(ps[bkk][:], W[:, c, 128:256], hnT[:, c, tbs(b)], c == 0, c == NCK - 1, [BW, rd_hn[c]], [PB[bkk]])
                S.add("vector", lambda e, k=k, bq=bq: e.scalar_tensor_tensor(out=qd[k][:], in0=ps[bq][:], scalar=GLA_SCALE, in1=eb[k][:],
                                                                             op0=ALU.mult, op1=ALU.mult),
                      reads=[PB[bq], BB["eb"][k]], writes=[BB["qd"][k]])
                S.add("vector", lambda e, k=k, bkk=bkk: e.tensor_tensor(out=kd[k][:], in0=ps[bkk][:], in1=enb[k][:], op=ALU.mult),
                      reads=[PB[bkk], BB["enb"][k]], writes=[BB["kd"][k]])
                S.add("gpsimd", lambda e, k=k: e.tensor_tensor(
                    out=kk[k][:].rearrange("p (c k) -> p c k", k=64), in0=kd[k][:].rearrange("p (c k) -> p c k", k=64),
                    in1=dec[k][:, 0:8].unsqueeze(2).to_broadcast([128, 8, 64]), op=ALU.mult),
                    reads=[BB["kd"][k], BB["dec"][k]], writes=[BB["kk"][k]])
                for tt in range(4):
                    bv = bank()
                    tok = slice(b * 512 + tt * 128, b * 512 + (tt + 1) * 128)
                    for c in range(NCK):
                        mm(ps[bv][:, 0:256], hnT[:, c, tok], W[:, c, 256:512], c == 0, c == NCK - 1, [BW, rd_hn[c]], [PB[bv]])
                    S.add("scalar", lambda e, k=k, bv=bv, tt=tt: e.copy(out=vt[k][:, tt, :], in_=ps[bv][:, 0:256]),
                          reads=[PB[bv]], writes=[BB["vt"][k]])
                for vc in range(2):
                    br = bank()
                    for c in range(NCK):
                        mm(ps[br][:], W[:, c, 512 + vc * 128:512 + (vc + 1) * 128], hnT[:, c, tbs(b)], c == 0, c == NCK - 1,
                           [BW, rd_hn[c]], [PB[br]])
                    S.add("scalar", lambda e, k=k, br=br, vc=vc: e.activation(out=sr[k][:, vc, :], in_=ps[br][:], func=AF.Silu),
                          reads=[PB[br]], writes=[BB["sr"][k]])

            def mid(it, h, b):
                k = it % 2
                hs = h % 2
                if b == 0:
                    S.add("gpsimd", lambda e: e.memset(Sf[0][:], 0.0), writes=[BSf[0]])
                    sfi[0] = 0
                bt = bank()
                for tt in range(4):
                    mm(ps[bt][:, tt * 128:(tt + 1) * 128], kk[k][:, tt * 128:(tt + 1) * 128], ident_bf[:], True, True,
                       [BB["kk"][k], Bconst], [PB[bt]])
                S.add("scalar", lambda e, k=k, bt=bt: e.copy(out=kktok[k][:].rearrange("p a b -> p (a b)"), in_=ps[bt][:]),
                      reads=[PB[bt]], writes=[BB["kktok"][k]])
                ba = bank()
                for tt in range(4):
                    mm(ps[ba][:, tt * 128:(tt + 1) * 128], kd[k][:, tt * 128:(tt + 1) * 128], qd[k][:, tt * 128:(tt + 1) * 128], True, True,
                       [BB["kd"][k], BB["qd"][k]], [PB[ba]])
                S.add("vector", lambda e, k=k, ba=ba: e.tensor_tensor(out=AT[k][:], in0=ps[ba][:], in1=maskbd[:], op=ALU.mult),
                      reads=[PB[ba], Bconst], writes=[BB["AT"][k]])
                for ci in range(8):
                    tt, hf = ci // 2, ci % 2
                    rows = slice(64 * hf, 64 * hf + 64)
                    bkv = bank()
                    mm(ps[bkv][:, 0:256], kktok[k][rows, tt, :], vt[k][rows, tt, :], True, True,
                       [BB["kktok"][k], BB["vt"][k]], [PB[bkv]])
                    so, sn = sfi[0], 1 - sfi[0]
                    S.add("vector", lambda e, k=k, ci=ci, bkv=bkv, so=so, sn=sn: e.scalar_tensor_tensor(
                        out=Sf[sn][:], in0=Sf[so][:], scalar=dec[k][:, ci:ci + 1], in1=ps[bkv][:, 0:256], op0=ALU.mult, op1=ALU.add),
                        reads=[BSf[so], BB["dec"][k], PB[bkv]], writes=[BSf[sn]])
                    sfi[0] = sn
                    if ci < 7:
                        S.add("scalar", lambda e, k=k, ci=ci, sn=sn: e.copy(out=Sall[k][:, ci + 1, :], in_=Sf[sn][:]),
                              reads=[BSf[sn]], writes=[BSall[k][ci + 1]])
                    elif b < NB - 1:
                        S.add("scalar", lambda e, it=it, sn=sn: e.copy(out=Sfin[it % 3][:], in_=Sf[sn][:]),
                              reads=[BSf[sn]], writes=[BSfin[it % 3]])

            def out(it, h, b):
                k = it % 2
                hs = h % 2
                bo = [bank(), bank()]
                for vc in range(2):
                    for tt in range(4):
                        mm(ps[bo[vc]][:, tt * 128:(tt + 1) * 128], vt[k][:, tt, vc * 128:(vc + 1) * 128], AT[k][:, tt * 128:(tt + 1) * 128],
                           True, False, [BB["vt"][k], BB["AT"][k]], [PB[bo[vc]]])
                        for hf in range(2):
                            ci = 2 * tt + hf
                            if ci == 0:
                                if b == 0:
                                    continue
                                st_ap, st_b = Sfin[(it - 1) % 3], BSfin[(it - 1) % 3]
                                lhs = st_ap[:, vc * 128:(vc + 1) * 128]
                            else:
                                st_b = BSall[k][ci]
                                lhs = Sall[k][:, ci, vc * 128:(vc + 1) * 128]
                            mm(ps[bo[vc]][:, ci * 64:(ci + 1) * 64], lhs, qd[k][:, ci * 64:(ci + 1) * 64],
                               False, hf == 1, [st_b, BB["qd"][k]], [PB[bo[vc]]])
                    s4 = (2 * it + vc) % 4
                    S.add("scalar", lambda e, s4=s4, bb=bo[vc]: e.activation(out=sq[s4][:], in_=ps[bb][:], func=AF.Square),
                          reads=[PB[bo[vc]]], writes=[Bsq[s4]])
                bn = bank()
                for vc in range(2):
                    s4 = (2 * it + vc) % 4
                    mm(ps[bn][:], ones_bf[:], sq[s4][:], vc == 0, vc == 1, [Bconst, Bsq[s4]], [PB[bn]])
                kr = nrm_ctr[0] % 2
                nrm_ctr[0] += 1
                S.add("scalar", lambda e, kr=kr, bn=bn: e.activation(out=rs_t[kr][:], in_=ps[bn][:], func=AF.Ln, scale=1.0 / 256, bias=EPS),
                      reads=[PB[bn]], writes=[Brs[kr]])
                S.add("scalar", lambda e, kr=kr: e.activation(out=rstd_t[kr][:], in_=rs_t[kr][:], func=AF.Exp, scale=-0.5), reads=[Brs[kr]], writes=[Brstd[kr]])
                for vc in range(2):
                    gi = GC_GHEAD + l * 2 + vc
                    S.add("vector", lambda e, k=k, vc=vc, kr=kr, gi=gi, bb=bo[vc]: e.scalar_tensor_tensor(
                        out=t0[k][:, vc, :], in0=ps[bb][:], scalar=gcol[:, gi:gi + 1], in1=rstd_t[kr][:], op0=ALU.mult, op1=ALU.mult),
                        reads=[PB[bo[vc]], Brstd[kr], Bconst], writes=[BB["t0"][k]])
                S.add("vector", lambda e, k=k: e.tensor_tensor(out=og[k][:].rearrange("p a b -> p (a b)"), in0=t0[k][:].rearrange("p a b -> p (a b)"),
                                                              in1=sr[k][:].rearrange("p a b -> p (a b)"), op=ALU.mult),
                      reads=[BB["t0"][k], BB["sr"][k]], writes=[BB["og"][k]])

            def out_b(it, h, b):
                k = it % 2
                hs = h % 2
                for c in range(NCK):
                    bw = bank()
                    for vc in range(2):
                        mm(ps[bw][:], wot[hs][:, vc, c * 128:(c + 1) * 128], og[k][:, vc, :], vc == 0, vc == 1,
                           [BB["wot"][hs], BB["og"][k]], [PB[bw]])
                    resid_add(c, b, bw)

            its = [(h, b) for h in range(4) for b in range(NB)]
            n_it = len(its)
            front(0, *its[0])
            if n_it > 1:
                front(1, *its[1])
            mid(0, *its[0])
            for i in range(n_it):
                out(i, *its[i])
                if i + 1 < n_it:
                    mid(i + 1, *its[i + 1])
                out_b(i, *its[i])
                if i + 2 < n_it:
                    front(i + 2, *its[i + 2])

        def kvproj():
            cv = Carver()
            wv = cv.take([128, NCK, 1024], BF16)
            wf = cv.take([128, NCK, 16], BF16)
            wk = [cv.take([128, NCK, 128], BF16) for _ in range(2)]
            kst = [cv.take([128, T], BF16) for _ in range(2)]
            vst = [cv.take([128, 16, 128], BF16) for _ in range(2)]
            lfp = cv.take([128, NT, 16], F32)
            ftmp = [cv.take([128, 16], F32) for _ in range(2)]
            Bwv, Bwf, Blfp = Buf("wv"), Buf("wf"), Buf("lfp")
            Bwk = [Buf("wk0"), Buf("wk1")]
            Bkst = [Buf("kst0"), Buf("kst1")]
            Bvst = [Buf("vst0"), Buf("vst1")]
            Bft = [Buf("ft0"), Buf("ft1")]
            norm(GC_KV)
            S.dma("gpsimd", wv[:], wv_d, Bwv, writes=[Bwv])
            S.dma("gpsimd", wf[:], wf_d, Bwf, writes=[Bwf])
            for i in range(2):
                S.add("gpsimd", lambda e, i=i: e.memset(vst[i][:], 1.0), writes=[Bvst[i]])
            for tt in range(NT):
                k = tt % 2
                tok = slice(tt * 128, (tt + 1) * 128)
                b = tt // 4
                bvs = [bank(), bank()]
                for half in range(2):
                    for c in range(NCK):
                        mm(ps[bvs[half]][:], hnT[:, c, tok], wv[:, c, half * 512:(half + 1) * 512], c == 0, c == NCK - 1,
                           [Bwv, Bhn[c][b]], [PB[bvs[half]]])
                bf_ = bank()
                for c in range(NCK):
                    mm(ps[bf_][:, 0:16], hnT[:, c, tok], wf[:, c, :], c == 0, c == NCK - 1, [Bwf, Bhn[c][b]], [PB[bf_]])
                for half in range(2):
                    src = ps[bvs[half]][:].rearrange("p (a two d) -> p a two d", two=2, d=64)
                    dst = vst[k][:, half * 8:(half + 1) * 8, :].rearrange("p (a two) d -> p a two d", two=2)
                    S.add("scalar", lambda e, src=src, dst=dst: e.copy(out=dst[:, :, 0, 0:64], in_=src[:, :, 0, :]),
                          reads=[PB[bvs[half]]], writes=[Bvst[k]])
                    S.add("vector", lambda e, src=src, dst=dst: e.tensor_copy(out=dst[:, :, 1, 64:128], in_=src[:, :, 1, :]),
                          reads=[PB[bvs[half]]], writes=[Bvst[k]])
                S.dma("sync", v_d[tok, :], vst[k][:].rearrange("p a b -> p (a b)"), Bvst[k], reads=[Bvst[k]], writes=[Bv_d])
                S.add("vector", lambda e, k=k, bf_=bf_: e.tensor_tensor(out=ftmp[k][:], in0=ps[bf_][:, 0:16], in1=bfb[:], op=ALU.add),
                      reads=[PB[bf_], Bconst], writes=[Bft[k]])
                S.add("scalar", lambda e, k=k: e.activation(out=ftmp[k][:], in_=ftmp[k][:], func=AF.Exp, scale=-1.0),
                      reads=[Bft[k]], writes=[Bft[k]])
                S.add("scalar", lambda e, k=k, tt=tt: e.activation(out=lfp[:, tt, :], in_=ftmp[k][:], func=AF.Ln, bias=1.0),
                      reads=[Bft[k]], writes=[Blfp])
            for tt in range(NT):
                bc = bank()
                mm(ps[bc][:, 0:16], U32[:], lfp[:, tt, :], True, tt == 0, [Bconst, Blfp], [PB[bc]])
                for t2 in range(tt):
                    mm(ps[bc][:, 0:16], ones32[:], lfp[:, t2, :], False, t2 == tt - 1, [Bconst, Blfp], [PB[bc]])
                S.add("vector", lambda e, bc=bc, tt=tt: e.tensor_copy(out=ctok[:, tt, :], in_=ps[bc][:, 0:16]), reads=[PB[bc]], writes=[Bctok])
            for qb in range(NB):
                bc = bank()
                mm(ps[bc][:, 0:16], E032[:], ctok[:, 4 * qb, :], True, True, [Bconst, Bctok], [PB[bc]])
                S.add("vector", lambda e, bc=bc, qb=qb: e.tensor_copy(out=cref[:, qb, :], in_=ps[bc][:, 0:16]), reads=[PB[bc]], writes=[Bcref])
                for kt in range(4 * qb + 4):
                    S.add("vector", lambda e, qb=qb, kt=kt: e.tensor_tensor(out=bias_t[:, kt, qb, :], in0=ctok[:, kt, :], in1=cref[:, qb, :], op=ALU.subtract),
                          reads=[Bctok, Bcref], writes=[Bbias])
            for hp in range(8):
                k = hp % 2
                S.dma("gpsimd", wk[k][:], wk_d[hp], Bwk[k], writes=[Bwk[k]])
                for b in range(NB):
                    bk = bank()
                    for c in range(NCK):
                        mm(ps[bk][:], wk[k][:, c, :], hnT[:, c, tbs(b)], c == 0, c == NCK - 1, [Bwk[k], Bhn[c][b]], [PB[bk]])
                    S.add("scalar", lambda e, k=k, bk=bk, b=b: e.copy(out=kst[k][:, tbs(b)], in_=ps[bk][:]), reads=[PB[bk]], writes=[Bkst[k]])
                S.dma("sync", kT_d[hp], kst[k][:], Bkst[k], reads=[Bkst[k]], writes=[Bk_d[hp]])

        def fox(l):
            j = l - 2
            cv = Carver()
            attn = cv.take([128, 8, T], BF16)
            cv.off = (cv.off + 31) // 32 * 32
            off_slot0 = cv.off
            QT, KT, Vh = [], [], []
            for _k in range(2):
                QT.append(cv.take([128, 2, T], BF16))
                KT.append(cv.take([128, T], BF16))
                Vh.append(cv.take([128, NT, 256], BF16))
                if _k == 0:
                    slot0_bytes = cv.off - off_slot0
            cvw = Carver()
            cvw.off = off_slot0
            wo = cvw.take([128, NCK, 1024], BF16)
            assert cvw.off - off_slot0 <= slot0_bytes, "wo must fit in the slot-0 group"
            Bwo = Buf("wo")
            wq = [cv.take([128, NCK, 128], BF16) for _ in range(2)]
            PT = [cv.take([128, 512], BF16) for _ in range(4)]
            rden = [cv.take([128, 512], F32) for _ in range(2)]
            BQT = [Buf("QT0"), Buf("QT1")]
            BKT = [Buf("KT0"), Buf("KT1")]
            BVh = [Buf("Vh0"), Buf("Vh1")]
            Bwq = [Buf("wq0"), Buf("wq1")]
            BPT = [Buf("PT%d" % i) for i in range(4)]
            Brden = [Buf("rden0"), Buf("rden1")]
            Battn = [[Buf("attn") for _ in range(NB)] for _ in range(8)]
            for k in range(2):
                S.add("gpsimd", lambda e, k=k: e.memset(QT[k][64:128, 0, :], 0.0), writes=[BQT[k]])
                S.add("gpsimd", lambda e, k=k: e.memset(QT[k][0:64, 1, :], 0.0), writes=[BQT[k]])
            norm(GC_ATTN + l * 8)
            pctr = 0
            qctr = 0
            def qproj(hp):
                k = hp % 2
                S.dma("gpsimd", wq[k][:], wq_d[j, hp], Bwq[k], writes=[Bwq[k]])
                S.dma("sync", KT[k][:], kT_d[hp], BKT[k], reads=[Bk_d[hp]], writes=[BKT[k]])
                S.dma("sync", Vh[k][:], v_d[:, hp * 256:(hp + 1) * 256].rearrange("(t p) c -> p t c", p=128), BVh[k],
                      reads=[Bv_d], writes=[BVh[k]])
                for b in range(NB):
                    bk = bank(4, 8)
                    for c in range(NCK):
                        mm(ps[bk][:], wq[k][:, c, :], hnT[:, c, tbs(b)], c == 0, c == NCK - 1, [Bwq[k], Bhn[c][b]], [PB[bk]])
                    for hh in range(2):
                        rows = slice(64 * hh, 64 * hh + 64)
                        S.add("scalar", lambda e, k=k, bk=bk, b=b, hh=hh, rows=rows: e.mul(out=QT[k][rows, hh, tbs(b)], in_=ps[bk][rows, :], mul=0.125),
                              reads=[PB[bk]], writes=[BQT[k]])

            qproj(0)
            for hp in range(8):
                k = hp % 2
                for qb in range(NB):
                    if qb == min(1, NB - 1) and hp == 7:
                        S.dma("gpsimd", wo[:], wo_d[j], Bwo, writes=[Bwo, BQT[0], BKT[0], BVh[0]])
                    if qb == min(1, NB - 1) and hp + 1 < 8:
                        qproj(hp + 1)
                    nk = 4 * qb + 4
                    ob = [2 * (qctr % 2), 2 * (qctr % 2) + 1]
                    qctr += 1
                    iters = [(kt, hh) for kt in range(nk) for hh in range(2)]
                    sbank = {}

                    def qk(i):
                        kt, hh = iters[i]
                        off = max(0, kt - 4 * qb) * 128
                        bs = bank(4, 8)
                        sbank[i] = bs
                        mm(ps[bs][:, off:512], KT[k][:, kt * 128:(kt + 1) * 128], QT[k][:, hh, qb * 512 + off:(qb + 1) * 512], True, True,
                           [BKT[k], BQT[k]], [PB[bs]])

                    def rest(i, pctr):
                        kt, hh = iters[i]
                        off = max(0, kt - 4 * qb) * 128
                        bs = sbank[i]
                        p = pctr % 4
                        hd = hp * 2 + hh
                        S.add("scalar", lambda e, p=p, bs=bs, off=off, kt=kt, qb=qb, hd=hd: e.activation(
                            out=PT[p][:, off:512], in_=ps[bs][:, off:512], func=AF.Exp, bias=bias_t[:, kt, qb, hd:hd + 1]),
                            reads=[PB[bs], Bbias], writes=[BPT[p]])
                        if kt >= 4 * qb:
                            S.add("gpsimd", lambda e, p=p, off=off: e.tensor_tensor(out=PT[p][:, off:off + 128], in0=PT[p][:, off:off + 128],
                                                                                  in1=tri[:], op=ALU.mult),
                                  reads=[BPT[p], Bconst], writes=[BPT[p]])
                        mm(ps[ob[hh]][:, off:512], Vh[k][:, kt, hh * 128:(hh + 1) * 128], PT[p][:, off:512], kt == 0, kt == nk - 1,
                           [BVh[k], BPT[p]], [PB[ob[hh]]])

                    LOOK = 3
                    for i in range(min(LOOK, len(iters))):
                        qk(i)
                    for i in range(len(iters)):
                        if i + LOOK < len(iters):
                            qk(i + LOOK)
                        rest(i, pctr)
                        pctr += 1
                    for hh in range(2):
                        num = slice(64 * hh, 64 * hh + 64)
                        den = slice(64 * (1 - hh), 64 * (1 - hh) + 64)
                        S.add("vector", lambda e, hh=hh, num=num, den=den, obk=ob[hh]: e.reciprocal(out=rden[hh][num, :], in_=ps[obk][den, :]),
                              reads=[PB[ob[hh]]], writes=[Brden[hh]])
                        S.add("vector", lambda e, hh=hh, num=num, hp=hp, qb=qb, obk=ob[hh]: e.tensor_tensor(out=attn[num, hp, tbs(qb)], in0=ps[obk][num, :],
                                                                                                          in1=rden[hh][num, :], op=ALU.mult),
                              reads=[PB[ob[hh]], Brden[hh]], writes=[Battn[hp][qb]])
            for c in range(NCK):
                for b in range(NB):
                    bk = bank()
                    for fc in range(8):
                        mm(ps[bk][:], wo[:, fc, c * 128:(c + 1) * 128], attn[:, fc, tbs(b)], fc == 0, fc == 7, [Bwo, Battn[fc][b]], [PB[bk]])
                    resid_add(c, b, bk)

        def final(s):
            cv = Carver()
            ystage = [cv.take([128, 512], F32) for _ in range(4)]
            Bys = [Buf("ys%d" % i) for i in range(4)]
            norm(GC_FINAL, final_seq=s, ystage=ystage, Bystage=Bys)

        consts()
        for s in range(NSEQ):
            for c in range(NCK):
                S.dma("sync", hT[:, c, :], xT_d[s, :, c, :], Bh[c][0], writes=Bh[c])
            for l in range(n_gla):
                S.barrier()
                gla(l)
                S.barrier()
                ffn(l)
            if n_fox > 0:
                S.barrier()
                kvproj()
            for l in range(2, 2 + n_fox):
                S.barrier()
                fox(l)
                S.barrier()
                ffn(l)
            S.barrier()
            if do_final:
                final(s)
            S.barrier()
        S.emit(nc, st)
    return nc, S


def prep_weights(inp):
    f = lambda a: np.ascontiguousarray(a, dtype=np.float32)
    col = lambda v: np.asarray(v, np.float32).reshape(-1, 128).T
    gcol = np.concatenate([
        col(inp["attn_norm"]), col(inp["ffn_norm"]), col(inp["kv_norm"]), col(inp["final_norm"]),
        col(inp["gla_b_alpha"]), col(inp["gla_g_head"])], axis=1)
    assert gcol.shape == (128, NGC)
    w_in = np.asarray(inp["gla_w_in"], np.float32)
    wr = w_in.reshape(2, NCK, 128, 3088)
    q = wr[..., 0:512].reshape(2, NCK, 128, 4, 128)
    k = wr[..., 512:1024].reshape(2, NCK, 128, 4, 128)
    v = wr[..., 1024:2048].reshape(2, NCK, 128, 4, 256)
    r = wr[..., 2048:3072].reshape(2, NCK, 128, 4, 256)
    slab = np.concatenate([q, k, v, r], axis=-1)
    wgla = slab.transpose(0, 3, 2, 1, 4)
    wa = wr[..., 3072:3088].transpose(0, 2, 1, 3)
    wout = np.asarray(inp["gla_w_out"], np.float32).reshape(2, 4, 2, 128, 1024).transpose(0, 1, 3, 2, 4)
    gu = np.asarray(inp["ffn_w_gu"], np.float32).reshape(4, NCK, 128, 2, NSLAB, 2, 128)
    wgu = gu.transpose(0, 4, 2, 1, 3, 5, 6).reshape(4, NSLAB, 128, NCK, 512)
    wd = np.asarray(inp["ffn_w_down"], np.float32).reshape(4, NJ, 128, 1024).transpose(0, 2, 1, 3)
    wkv = np.asarray(inp["w_kv"], np.float32).reshape(NCK, 128, 2064)
    wk = wkv[..., 0:1024].reshape(NCK, 128, 8, 128).transpose(2, 1, 0, 3)
    wv = wkv[..., 1024:2048].transpose(1, 0, 2)
    wf = wkv[..., 2048:2064].transpose(1, 0, 2)
    wq = np.asarray(inp["fox_w_q"], np.float32).reshape(2, NCK, 128, 8, 128).transpose(0, 3, 2, 1, 4)
    wo = np.asarray(inp["fox_w_o"], np.float32).reshape(2, NCK, 128, 1024).transpose(0, 2, 1, 3)
    bfb = np.tile(np.asarray(inp["b_f"], np.float32)[None, :], (128, 1))
    return {"gcol": f(gcol), "bfb": f(bfb), "wgla": f(wgla), "wout": f(wout), "wa": f(wa),
            "wup": f(inp["gla_w_alpha_up"]), "wgu": f(wgu), "wd": f(wd), "wk": f(wk), "wv": f(wv),
            "wf": f(wf), "wq": f(wq), "wo": f(wo)}


def prep_x(xs):
    n, T, _ = xs.shape
    return np.ascontiguousarray(xs.reshape(n, T, NCK, 128).transpose(0, 3, 2, 1), dtype=np.float32)


def unprep_y(yT):
    n, _, _, T = yT.shape
    return np.ascontiguousarray(yT.transpose(0, 3, 2, 1).reshape(n, T, D))


_NC_CACHE = {}
N_CORES = 8


def kernel(**inputs):
    x = np.asarray(inputs["x"], np.float32)
    B, T, _ = x.shape
    n_cores = N_CORES
    per = B // n_cores
    key = (T, per)
    if key not in _NC_CACHE:
        _NC_CACHE[key] = build_program(T=T, NSEQ=per)[0]
    nc = _NC_CACHE[key]
    w = prep_weights(inputs)
    in_maps = []
    for c in range(n_cores):
        m = dict(w)
        m["xT"] = prep_x(x[c * per:(c + 1) * per])
        in_maps.append(m)
    res = run_bass_kernel_spmd(nc, in_maps, core_ids=list(range(n_cores)))
    out = np.concatenate([unprep_y(r["yT"]) for r in res.results], axis=0)
    return out.astype(np.float32)
```
